# Optimizing a Trainium2 kernel written in Bass

```python
import jax, jax.numpy as jnp
from jax import lax
import numpy as np

D_MODEL = 4096
BATCH = 8
SEQ = 2048
DEPTH = 2

POOL_WINDOWS = (2, 4, 8, 16)
POOL_GROUPS = 4
POOL_GROUP_DIM = D_MODEL // 16
POOL_WIDTH = POOL_GROUPS * POOL_GROUP_DIM
SB_HEAD_DIM = 128
SB_WIDTH = 3 * D_MODEL // 8
SB_HEADS = SB_WIDTH // SB_HEAD_DIM
SB_BLOCK = 128
HG_DK = 128
HG_DV = 128
HG_WIDTH = 3 * D_MODEL // 8
HG_HEADS = HG_WIDTH // HG_DK
HG_CHUNK = 64
N_BRANCH = 3
MIX_WIDTH = POOL_WIDTH + SB_WIDTH + HG_WIDTH
IN_WIDTH = POOL_WIDTH + 3 * SB_WIDTH + 4 * HG_WIDTH + N_BRANCH * D_MODEL
SPLIT_POINTS = (POOL_WIDTH,
                POOL_WIDTH + SB_WIDTH,
                POOL_WIDTH + 2 * SB_WIDTH,
                POOL_WIDTH + 3 * SB_WIDTH,
                POOL_WIDTH + 3 * SB_WIDTH + HG_WIDTH,
                POOL_WIDTH + 3 * SB_WIDTH + 2 * HG_WIDTH,
                POOL_WIDTH + 3 * SB_WIDTH + 3 * HG_WIDTH,
                POOL_WIDTH + 3 * SB_WIDTH + 4 * HG_WIDTH)
D_FF = 11008
N_EXPERTS = 8
TOP_K = 2
D_EXPERT = 3072
N_DENSE = (DEPTH + 1) // 2
N_MOE = DEPTH // 2
N_MOD = 6
EPS = 1e-6

kernel_name = 'gated_hybrid_pool_stickbreak_hgrn2_moe'


def rms_norm(x, gain):
    xf = x.astype(jnp.float32)
    y = xf * lax.rsqrt(jnp.mean(xf * xf, axis=-1, keepdims=True) + EPS)
    return (y * gain.astype(jnp.float32)).astype(x.dtype)


def swiglu(h, w1, w3, w2):
    return (jax.nn.silu(h @ w1) * (h @ w3)) @ w2


def pool_mixer(u, w_pool, pool_scale):
    B, S, _ = u.shape
    ug = u.astype(jnp.float32).reshape(B, S, POOL_GROUPS, POOL_GROUP_DIM)
    cs = jnp.cumsum(ug, axis=1)
    pos = jnp.arange(S)
    pooled = []
    for g, w in enumerate(POOL_WINDOWS):
        c_g = cs[:, :, g]
        lower = jnp.pad(c_g, ((0, 0), (w, 0), (0, 0)))[:, :S]
        count = jnp.minimum(pos + 1, w).astype(jnp.float32)[None, :, None]
        pooled.append((c_g - lower) / count)
    pooled = jnp.stack(pooled, axis=2) - ug
    mixed = jnp.einsum('bsgc,gcd->bsgd', pooled.astype(u.dtype), w_pool)
    return mixed.reshape(B, S, POOL_WIDTH) * pool_scale


def stick_breaking_attention(q, k, v):
    B, H, S, Dh = q.shape
    scale = Dh ** -0.5
    outs = []
    for blk in range(S // SB_BLOCK):
        t0, t1 = blk * SB_BLOCK, (blk + 1) * SB_BLOCK
        qb, kb, vb = q[:, :, t0:t1], k[:, :, :t1], v[:, :, :t1]
        z = jnp.einsum('bhtd,bhsd->bhts', qb, kb).astype(jnp.float32) * scale
        mask = jnp.arange(t1)[None, :] < jnp.arange(t0, t1)[:, None]
        log_beta = jax.nn.log_sigmoid(z)
        log_keep = jnp.where(mask, jax.nn.log_sigmoid(-z), 0.0)
        after = lax.cumsum(log_keep, axis=3, reverse=True) - log_keep
        a = jnp.where(mask, jnp.exp(log_beta + after), 0.0)
        outs.append(jnp.einsum('bhts,bhsd->bhtd', a.astype(v.dtype), vb))
    return jnp.concatenate(outs, axis=2)


def hgrn2_recurrence(q, k, i, log_f):
    B, S, H, DK = q.shape
    DV = i.shape[-1]
    N = S // HG_CHUNK

    def to_chunks(t):
        return t.astype(jnp.float32).reshape(B, N, HG_CHUNK, H, t.shape[-1]).transpose(1, 0, 3, 2, 4)

    qc, kc, ic, fc = to_chunks(q), to_chunks(k), to_chunks(i), to_chunks(log_f)
    causal = jnp.tril(jnp.ones((HG_CHUNK, HG_CHUNK), dtype=bool))[None, None, :, :, None]

    def step(state, xs):
        qx, kx, ix, fx = xs
        b = jnp.cumsum(fx, axis=2)
        diff = b[:, :, :, None, :] - b[:, :, None, :, :]
        decay = jnp.exp(jnp.where(causal, diff, -jnp.inf))
        scores = jnp.einsum('bhtk,bhsk,bhtsk->bhts', qx, kx, decay)
        o = (jnp.einsum('bhts,bhsv->bhtv', scores, ix)
             + jnp.einsum('bhtk,bhkv->bhtv', qx * jnp.exp(b), state))
        b_last = b[:, :, -1:, :]
        state = (jnp.exp(b_last[:, :, 0, :, None]) * state
                 + jnp.einsum('bhsk,bhsv->bhkv', kx * jnp.exp(b_last - b), ix))
        return state, o

    s0 = jnp.zeros((B, H, DK, DV), jnp.float32)
    _, o = lax.scan(step, s0, (qc, kc, ic, fc))
    return o.transpose(1, 0, 3, 2, 4).reshape(B, S, H, DV)


def hybrid_mixer(h, w_in, w_pool, pool_scale, lower_bound, hg_norm, w_br_pool, w_br_sb, w_br_hg, w_out):
    B, S, _ = h.shape
    proj = h @ w_in
    u_pool, q_sb, k_sb, v_sb, q_hg, f_hg, i_hg, g_hg, gate_logits = jnp.split(proj, SPLIT_POINTS, axis=-1)

    y_pool = pool_mixer(u_pool, w_pool, pool_scale)

    def heads(t):
        return t.reshape(B, S, SB_HEADS, SB_HEAD_DIM).transpose(0, 2, 1, 3)
    o_sb = stick_breaking_attention(heads(q_sb), heads(k_sb), heads(v_sb))
    y_sb = o_sb.transpose(0, 2, 1, 3).reshape(B, S, SB_WIDTH)

    f = lower_bound + (1.0 - lower_bound) * jax.nn.sigmoid(f_hg.astype(jnp.float32))
    log_f = jnp.log(f)
    k_hg = 1.0 - f
    q_act = jax.nn.silu(q_hg)
    o_hg = hgrn2_recurrence(q_act.reshape(B, S, HG_HEADS, HG_DK),
                            k_hg.reshape(B, S, HG_HEADS, HG_DK),
                            i_hg.reshape(B, S, HG_HEADS, HG_DV),
                            log_f.reshape(B, S, HG_HEADS, HG_DK)).astype(h.dtype)
    o_hg = rms_norm(o_hg, hg_norm) * jax.nn.silu(g_hg.reshape(B, S, HG_HEADS, HG_DV))
    y_hg = o_hg.reshape(B, S, HG_WIDTH)

    gate = jax.nn.sigmoid(gate_logits.reshape(B, S, N_BRANCH, D_MODEL))
    merged = (gate[:, :, 0] * (y_pool @ w_br_pool)
              + gate[:, :, 1] * (y_sb @ w_br_sb)
              + gate[:, :, 2] * (y_hg @ w_br_hg))
    return merged @ w_out


def moe_swiglu(h, w_router, b_router, w1, w3, w2):
    logits = (h @ w_router).astype(jnp.float32) + b_router.astype(jnp.float32)
    top_v, top_i = lax.top_k(logits, TOP_K)
    top_w = jax.nn.softmax(top_v, axis=-1)
    combine = jnp.sum(jax.nn.one_hot(top_i, N_EXPERTS, dtype=jnp.float32) * top_w[..., None], axis=-2)
    y = jnp.zeros_like(h)
    for e in range(N_EXPERTS):
        y = y + combine[..., e:e + 1].astype(h.dtype) * swiglu(h, w1[e], w3[e], w2[e])
    return y


def setup_inputs(seed: int = 0) -> dict:
    key = jax.random.key(seed)
    ks = jax.random.split(key, 32)
    f32 = jnp.float32

    def nrm(k, shape, scale):
        return jax.random.normal(k, shape, f32) * scale

    def gain(k, shape):
        return 1.0 + 0.02 * jax.random.normal(k, shape, f32)

    D = D_MODEL
    return {
        'x': nrm(ks[0], (BATCH, SEQ, D), 1.0),
        'c': nrm(ks[1], (BATCH, D), 1.0),
        'w_ada': nrm(ks[2], (DEPTH, D, N_MOD * D), 0.5 * D ** -0.5),
        'b_ada': nrm(ks[3], (DEPTH, N_MOD * D), 0.02),
        'norm_mix': gain(ks[4], (DEPTH, D)),
        'norm_ffn': gain(ks[5], (DEPTH, D)),
        'w_in': nrm(ks[6], (DEPTH, D, IN_WIDTH), D ** -0.5),
        'w_pool': nrm(ks[7], (DEPTH, POOL_GROUPS, POOL_GROUP_DIM, POOL_GROUP_DIM), POOL_GROUP_DIM ** -0.5),
        'pool_scale': gain(ks[8], (DEPTH, POOL_WIDTH)),
        'lb_logits': nrm(ks[9], (DEPTH, HG_WIDTH), 0.5),
        'hg_norm': gain(ks[10], (DEPTH, HG_DV)),
        'w_br_pool': nrm(ks[11], (DEPTH, POOL_WIDTH, D), POOL_WIDTH ** -0.5),
        'w_br_sb': nrm(ks[12], (DEPTH, SB_WIDTH, D), SB_WIDTH ** -0.5),
        'w_br_hg': nrm(ks[13], (DEPTH, HG_WIDTH, D), HG_WIDTH ** -0.5),
        'w_out': nrm(ks[14], (DEPTH, D, D), D ** -0.5),
        'ffn_w1': nrm(ks[15], (N_DENSE, D, D_FF), D ** -0.5),
        'ffn_w3': nrm(ks[16], (N_DENSE, D, D_FF), D ** -0.5),
        'ffn_w2': nrm(ks[17], (N_DENSE, D_FF, D), D_FF ** -0.5),
        'w_router': nrm(ks[18], (N_MOE, D, N_EXPERTS), D ** -0.5),
        'b_router': nrm(ks[19], (N_MOE, N_EXPERTS), 0.01),
        'moe_w1': nrm(ks[20], (N_MOE, N_EXPERTS, D, D_EXPERT), D ** -0.5),
        'moe_w3': nrm(ks[21], (N_MOE, N_EXPERTS, D, D_EXPERT), D ** -0.5),
        'moe_w2': nrm(ks[22], (N_MOE, N_EXPERTS, D_EXPERT, D), D_EXPERT ** -0.5),
        'final_norm': gain(ks[23], (D,)),
    }


def reference(x, c, w_ada, b_ada, norm_mix, norm_ffn, w_in, w_pool, pool_scale, lb_logits, hg_norm,
              w_br_pool, w_br_sb, w_br_hg, w_out, ffn_w1, ffn_w3, ffn_w2, w_router, b_router,
              moe_w1, moe_w3, moe_w2, final_norm):
    lb_p = jax.nn.softmax(lb_logits.astype(jnp.float32), axis=0)
    lower_bounds = jnp.cumsum(lb_p, axis=0) - lb_p[0:1]
    c_act = jax.nn.silu(c)
    for l in range(DEPTH):
        mod = c_act @ w_ada[l] + b_ada[l]
        sh1, sc1, gt1, sh2, sc2, gt2 = jnp.split(mod[:, None, :], N_MOD, axis=-1)
        h = rms_norm(x, norm_mix[l]) * (1.0 + sc1) + sh1
        x = x + gt1 * hybrid_mixer(h, w_in[l], w_pool[l], pool_scale[l], lower_bounds[l], hg_norm[l],
                                   w_br_pool[l], w_br_sb[l], w_br_hg[l], w_out[l])
        h = rms_norm(x, norm_ffn[l]) * (1.0 + sc2) + sh2
        if l % 2 == 0:
            j = l // 2
            f_out = swiglu(h, ffn_w1[j], ffn_w3[j], ffn_w2[j])
        else:
            j = l // 2
            f_out = moe_swiglu(h, w_router[j], b_router[j], moe_w1[j], moe_w3[j], moe_w2[j])
        x = x + gt2 * f_out
    return rms_norm(x, final_norm)
```

```python
import numpy as np
from contextlib import ExitStack
import concourse.bass as bass
import concourse.mybir as mybir
from concourse.bass_utils import run_bass_kernel_spmd

F32 = mybir.dt.float32
BF16 = mybir.dt.bfloat16
AF = mybir.ActivationFunctionType
ALU = mybir.AluOpType
EPS = 1e-6
POOL_WINDOWS = (2, 4, 8, 16)


class Cfg:
    def __init__(s, D=4096, S=2048, NB=2, DFF=11008, NE=8, DE=3072, L=2, debug=False):
        s.D, s.S, s.NB, s.DFF, s.NE, s.DE, s.L, s.debug = D, S, NB, DFF, NE, DE, L, debug
        s.KC = D // 128
        s.PG = D // 16
        s.PW = 4 * s.PG
        s.SBW = 3 * D // 8
        s.H = s.SBW // 128
        s.HGW = s.SBW
        s.INW = s.PW + 3 * s.SBW + 4 * s.HGW + 3 * D
        s.o_q = s.PW
        s.o_k = s.o_q + s.SBW
        s.o_v = s.o_k + s.SBW
        s.o_qh = s.o_v + s.SBW
        s.o_fh = s.o_qh + s.HGW
        s.o_ih = s.o_fh + s.HGW
        s.o_gh = s.o_ih + s.HGW
        s.o_gate = s.o_gh + s.HGW
        s.NT = S // 512
        s.ND = (L + 1) // 2
        s.NM = L // 2
        s.HID = max(DFF, NE * DE)
        s.c_ident = 0
        s.c_tri = 128
        s.c_ones = 256
        s.c_hgm = 384
        s.c_att = 512
        s.c_scan = 512 + 2048
        s.c_inv = s.c_scan + S
        s.NCONST = s.c_inv + 4 * S


def make_consts(cfg):
    S = cfg.S
    c = np.zeros((128, cfg.NCONST), np.float32)
    p = np.arange(128)[:, None]
    j = np.arange(128)[None, :]
    c[:, 0:128] = (p == j)
    c[:, 128:256] = (p > j)
    c[:, 256:384] = 1.0
    c[:, 384:512] = ((p // 64) == (j // 64)) & (p <= j)
    jj = np.arange(512)[None, :]
    for d in range(4):
        c[:, 512 + d * 512: 512 + (d + 1) * 512] = (jj > d * 128 + p)
    t = np.arange(S)
    c[:, cfg.c_scan:cfg.c_scan + S] = (t % 64 != 0)[None, :]
    for g, w in enumerate(POOL_WINDOWS):
        c[:, cfg.c_inv + g * S: cfg.c_inv + (g + 1) * S] = (1.0 / np.minimum(t + 1, w))[None, :]
    return c


class Sem:
    def __init__(s, h):
        s.h = h
        s.n = 0


class Prog:
    def __init__(s, nc, es):
        s.nc = nc
        s.es = es
        s.engs = {'pe': nc.tensor, 'act': nc.scalar, 'dve': nc.vector, 'pool': nc.gpsimd, 'sp': nc.sync}
        s.nsem = 0
        s.allsems = []
        s.freelist = []
        s.taken = []
        s.clk = {e: s.sem("clk_" + e) for e in ['pe', 'act', 'dve', 'pool']}
        s.waited = {}
        s.uid = 0

    def sem(s, name=None):
        if s.freelist:
            sm = s.freelist.pop()
        else:
            s.nsem += 1
            sm = Sem(s.es.enter_context(s.nc.semaphore("s%d_%s" % (s.nsem, name or ""))))
            s.allsems.append(sm)
        s.taken.append(sm)
        return sm

    def mark(s):
        return len(s.taken)

    def release(s, mark):
        while len(s.taken) > mark:
            s.freelist.append(s.taken.pop())

    def name(s, base):
        s.uid += 1
        return "%s_%d" % (base, s.uid)

    def wait(s, eng, tok):
        if tok is None:
            return
        sem, val = tok
        if val <= 0:
            return
        key = (eng, id(sem))
        if s.waited.get(key, 0) >= val:
            return
        s.waited[key] = val
        s.engs[eng].wait_ge(sem.h, val)

    def op(s, eng, fn, deps=(), sig=True):
        for d in deps:
            s.wait(eng, d)
        ins = fn(s.engs[eng])
        if sig:
            c = s.clk[eng]
            c.n += 1
            ins.then_inc(c.h, 1)
            return (c, c.n)
        return None

    def dma(s, q, out, in_, sem, deps=()):
        for d in deps:
            s.wait(q, d)
        sem.n += 16
        s.engs[q].dma_start(out=out, in_=in_).then_inc(sem.h, 16)
        return (sem, sem.n)

    def barrier(s):
        toks = [(c, c.n) for c in s.allsems]
        for e in s.engs:
            for t in toks:
                s.wait(e, t)


class Ring:
    def __init__(s, P, es, name, shape, dtype, n, with_sems=True):
        s.n = n
        s.bufs = [es.enter_context(P.nc.sbuf_tensor(P.name(name), shape, dtype)) for _ in range(n)]
        s.sems = [P.sem(name) for _ in range(n)] if with_sems else None
        s.free = [None] * n
        s.i = 0

    def next(s):
        k = s.i % s.n
        s.i += 1
        return k, s.bufs[k], s.free[k]


def build(cfg):
    nc = bass.Bass("TRN2", target_bir_lowering=False)
    D, S, NB, KC, L = cfg.D, cfg.S, cfg.NB, cfg.KC, cfg.L
    H, PW, SBW, HGW, INW = cfg.H, cfg.PW, cfg.SBW, cfg.HGW, cfg.INW
    NT = cfg.NT

    def din(name, shape):
        return nc.dram_tensor(name, list(shape), F32, kind="ExternalInput")

    x_in = din("x", [NB, S, D])
    c_in = din("c", [NB, D])
    w_ada = din("w_ada", [L, D, 6 * D])
    b_ada = din("b_ada", [L, 6 * D])
    norm_mix = din("norm_mix", [L, D])
    norm_ffn = din("norm_ffn", [L, D])
    w_in = din("w_in", [L, D, INW])
    w_pool = din("w_pool", [L, 4, cfg.PG, cfg.PG])
    pool_scale = din("pool_scale", [L, PW])
    lb_logits = din("lb_logits", [L, HGW])
    hg_norm = din("hg_norm", [L, 128])
    w_br_pool = din("w_br_pool", [L, PW, D])
    w_br_sb = din("w_br_sb", [L, SBW, D])
    w_br_hg = din("w_br_hg", [L, HGW, D])
    w_out = din("w_out", [L, D, D])
    ffn_w1 = din("ffn_w1", [cfg.ND, D, cfg.DFF])
    ffn_w3 = din("ffn_w3", [cfg.ND, D, cfg.DFF])
    ffn_w2 = din("ffn_w2", [cfg.ND, cfg.DFF, D])
    w_router = din("w_router", [max(cfg.NM, 1), D, cfg.NE])
    b_router = din("b_router", [max(cfg.NM, 1), cfg.NE])
    moe_w1 = din("moe_w1", [max(cfg.NM, 1), cfg.NE, D, cfg.DE])
    moe_w3 = din("moe_w3", [max(cfg.NM, 1), cfg.NE, D, cfg.DE])
    moe_w2 = din("moe_w2", [max(cfg.NM, 1), cfg.NE, cfg.DE, D])
    final_norm = din("final_norm", [D])
    consts = din("consts", [128, cfg.NCONST])
    out = nc.dram_tensor("out", [NB, S, D], F32, kind="ExternalOutput")

    skind = "ExternalOutput" if cfg.debug else "Internal"

    def scratch(name, shape, dt):
        if cfg.debug:
            return nc.dram_tensor(name, list(shape), dt, kind="ExternalOutput")
        return nc.dram_tensor(name, list(shape), dt)

    xT = scratch("xT", [D, S], F32)
    proj = scratch("proj", [INW, S], F32)
    ymix = scratch("ymix", [D, S], BF16)
    merged = scratch("merged", [D, S], BF16)
    hid = scratch("hid", [cfg.HID, S], BF16)

    es = ExitStack()
    with es:
        P = Prog(nc, es)

        def sb(name, shape, dt, stack=None):
            return (stack or es).enter_context(nc.sbuf_tensor(P.name(name), list(shape), dt))

        def ps(name, shape, dt, stack):
            return stack.enter_context(nc.psum_tensor(P.name(name), list(shape), dt))

        from contextlib import contextmanager

        @contextmanager
        def phase():
            mk = P.mark()
            with ExitStack() as st_:
                yield st_
                P.barrier()
            P.release(mk)

        cst = sb("cst", [128, 512 + 2048], F32)
        identb = sb("identb", [128, 128], BF16)
        csem = P.sem("c")
        t_c = P.dma('sp', cst[:], consts[:, 0:512 + 2048], csem)
        ident = cst[:, 0:128]
        tri = cst[:, 128:256]
        ones = cst[:, 256:384]
        hgm = cst[:, 384:512]
        attm = [cst[:, 512 + d * 512: 512 + (d + 1) * 512] for d in range(4)]
        t_ib = P.op('dve', lambda e: e.tensor_copy(identb[:], ident), deps=[t_c])
        hgmi = sb("hgmi", [128, 128], mybir.dt.int32)
        t_hi = P.op('dve', lambda e: e.tensor_copy(hgmi[:], hgm), deps=[t_c])

        NJ = 6 * KC
        modv = sb("modv", [128, L, NJ, NB], F32)
        gmix = sb("gmix", [128, L, KC], F32)
        gffn = sb("gffn", [128, L, KC], F32)
        gfin = sb("gfin", [128, KC], F32)
        pscale = sb("pscale", [128, L, PW // 128], F32)
        lbt = sb("lbt", [128, L, H], F32)
        oml = sb("oml", [128, L, H], F32)
        hgn = sb("hgn", [128, L], F32)
        badat = sb("badat", [128, L, NJ], F32)
        modA = sb("modA", [128, L, 2, NB, KC], F32)
        cact = sb("cact", [128, NB, KC], F32)

        with phase() as st:
            rowb = Ring(P, st, "rowb", [128, 128], F32, 2)
            pst = ps("pst", [128, 2, 512], F32, st)
            pfree = [None, None]
            cnt = [0]

            def vec_to_fm(src2d, n, dst, func=None):
                k, rb, fr = rowb.next()
                t1 = P.dma('sp', rb[0:n, :], src2d, rowb.sems[k], deps=[fr])
                j = cnt[0] % 2
                cnt[0] += 1
                t2 = P.op('pe', lambda e: e.transpose(pst[:, j, 0:n], rb[0:n, :], ident[0:n, 0:n]),
                          deps=[t1, t_c, pfree[j]])
                if func is None:
                    t3 = P.op('dve', lambda e: e.tensor_copy(dst, pst[:, j, 0:n]), deps=[t2])
                else:
                    t3 = P.op('act', lambda e: e.activation(out=dst, in_=pst[:, j, 0:n], func=func), deps=[t2])
                pfree[j] = t3
                rowb.free[k] = t2
                return t3

            def v2(ap1d, n):
                return ap1d.rearrange("(k p) -> k p", p=128)

            toks = []
            for l in range(L):
                toks.append(vec_to_fm(v2(norm_mix[l, :], KC), KC, gmix[:, l, :]))
                toks.append(vec_to_fm(v2(norm_ffn[l, :], KC), KC, gffn[:, l, :]))
                toks.append(vec_to_fm(v2(pool_scale[l, :], PW // 128), PW // 128, pscale[:, l, :]))
                toks.append(vec_to_fm(v2(lb_logits[l, :], H), H, lbt[:, l, :]))
                toks.append(vec_to_fm(hg_norm[l:l + 1, :], 1, hgn[:, l:l + 1]))
                for j0 in range(0, NJ, 128):
                    n = min(128, NJ - j0)
                    toks.append(vec_to_fm(b_ada[l, j0 * 128:(j0 + n) * 128].rearrange("(k p) -> k p", p=128), n,
                                          badat[:, l, j0:j0 + n]))
            toks.append(vec_to_fm(v2(final_norm[:], KC), KC, gfin[:, :]))
            for b in range(NB):
                toks.append(vec_to_fm(v2(c_in[b, :], KC), KC, cact[:, b, :], func=AF.Silu))
            with phase() as st2:
                ex = sb("lbex", [128, L, H], F32, st2)
                sm = sb("lbsm", [128, H], F32, st2)
                tl = None
                for l in range(L):
                    tl = P.op('act', lambda e: e.activation(out=ex[:, l, :], in_=lbt[:, l, :], func=AF.Exp),
                              deps=toks + [tl])
                tl = P.op('dve', lambda e: e.tensor_copy(sm[:], ex[:, 0, :]), deps=[tl])
                for l in range(1, L):
                    tl = P.op('dve', lambda e: e.tensor_tensor(out=sm[:], in0=sm[:], in1=ex[:, l, :], op=ALU.add), deps=[tl])
                tl = P.op('dve', lambda e: e.reciprocal(sm[:], sm[:]), deps=[tl])
                tl = P.op('dve', lambda e: e.memset(lbt[:, 0, :], 0.0), deps=[tl])
                for l in range(1, L):
                    tl = P.op('dve', lambda e: e.tensor_tensor(out=ex[:, l, :], in0=ex[:, l, :], in1=sm[:], op=ALU.mult), deps=[tl])
                    tl = P.op('dve', lambda e: e.tensor_tensor(out=lbt[:, l, :], in0=lbt[:, l - 1, :], in1=ex[:, l, :], op=ALU.add), deps=[tl])
                tl = P.op('dve', lambda e: e.tensor_scalar(out=oml[:], in0=lbt[:], scalar1=-1.0, scalar2=1.0,
                                                           op0=ALU.mult, op1=ALU.add), deps=[tl])
                P.barrier()

            WB = 256
            wring = Ring(P, st, "wada", [128, KC, WB], F32, 2)
            pmod = ps("pmod", [128, 256, 4], F32, st)
            for l in range(L):
                tlast = None
                for c0 in range(0, 6 * D, WB):
                    k, wb_, fr = wring.next()
                    t1 = P.dma('sp', wb_[:], w_ada[l, :, c0:c0 + WB].rearrange("(k p) n -> p k n", p=128),
                               wring.sems[k], deps=[fr])
                    for m in range(WB // 128):
                        j = c0 // 128 + m
                        for kc in range(KC):
                            tlast = P.op('pe', lambda e: e.matmul(pmod[:, j, 0:NB], wb_[:, kc, m * 128:(m + 1) * 128],
                                                                  cact[:, :, kc], start=(kc == 0), stop=(kc == KC - 1)),
                                         deps=[t1] + toks, sig=(kc == KC - 1))
                    wring.free[k] = tlast
                for b in range(NB):
                    P.op('dve', lambda e: e.tensor_tensor(out=modv[:, l, :, b], in0=pmod[:, 0:NJ, b], in1=badat[:, l, :],
                                                          op=ALU.add), deps=[tlast])
                P.barrier()
            for l in range(L):
                for w in range(2):
                    g = gmix if w == 0 else gffn
                    for b in range(NB):
                        P.op('dve', lambda e: e.scalar_tensor_tensor(
                            out=modA[:, l, w, b, :], in0=modv[:, l, (3 * w + 1) * KC:(3 * w + 2) * KC, b], scalar=1.0,
                            in1=g[:, l, :], op0=ALU.add, op1=ALU.mult))
            P.barrier()

        def mod_shift(l, w, b):
            return lambda kc: modv[:, l, (3 * w) * KC + kc, b:b + 1]

        def mod_gate(l, w, b):
            return lambda kc: modv[:, l, (3 * w + 2) * KC + kc, b:b + 1]

        def norm_loader(st, scaleA, shiftB, router=None):
            xt = sb("xt", [128, KC, 512], F32, st)
            sqr = Ring(P, st, "sq", [128, 512], F32, 2, with_sems=False)
            rstd = sb("rstd", [128, 512], F32, st)
            psn = ps("psn", [128, 512], F32, st)
            xsem = P.sem("xt")
            state = {'xfree': []}

            def load(tt, hb, deps):
                tok = slice(tt * 512, (tt + 1) * 512)
                t_ld = None
                for k0 in range(0, KC, 16):
                    k1 = min(KC, k0 + 16)
                    t_ld = P.dma('sp', xt[:, k0:k1, :], xT[k0 * 128:k1 * 128, tok].rearrange("(k p) t -> p k t", p=128), xsem,
                                 deps=state['xfree'] if k0 == 0 else ())
                tm = None
                for kc in range(KC):
                    k, sq, fr = sqr.next()
                    t1 = P.op('act', lambda e: e.activation(out=sq[:], in_=xt[:, kc, :], func=AF.Square), deps=[t_ld, fr])
                    tm = P.op('pe', lambda e: e.matmul(psn[:], ones, sq[:], start=(kc == 0), stop=(kc == KC - 1)),
                              deps=[t1, t_c])
                    sqr.free[k] = tm
                t2 = P.op('dve', lambda e: e.tensor_scalar(out=rstd[:], in0=psn[:], scalar1=1.0 / D, scalar2=EPS,
                                                           op0=ALU.mult, op1=ALU.add), deps=[tm])
                t2 = P.op('act', lambda e: e.activation(out=rstd[:], in_=rstd[:], func=AF.Sqrt), deps=[t2])
                t2 = P.op('dve', lambda e: e.reciprocal(rstd[:], rstd[:]), deps=[t2])
                tl = []
                for kc in range(KC):
                    t3 = P.op('dve', lambda e: e.tensor_tensor(out=xt[:, kc, :], in0=xt[:, kc, :], in1=rstd[:], op=ALU.mult),
                              deps=[t2])
                    t4 = P.op('act', lambda e: e.activation(out=xt[:, kc, :], in_=xt[:, kc, :], func=AF.Identity,
                                                            scale=scaleA(kc), bias=shiftB(kc)), deps=[t3])
                    tl.append(t4)
                t5 = None
                NPc = 4 if KC >= 4 else 1
                stp = KC // NPc
                for i in range(NPc):
                    t5 = P.op('pool', lambda e: e.tensor_copy(hb[:, i * stp:(i + 1) * stp, :], xt[:, i * stp:(i + 1) * stp, :]),
                              deps=[tl[(i + 1) * stp - 1]] + list(deps))
                state['xfree'] = [t5]
                if router is not None:
                    tr = router(tt, xt, tl[-1])
                    state['xfree'] = [t5, tr]
                return t5
            return load

        def dram_loader(st, src, KCin):
            sem = P.sem("hl")

            def load(tt, hb, deps):
                tok = slice(tt * 512, (tt + 1) * 512)
                t = None
                for k0 in range(0, KCin, 16):
                    k1 = min(KCin, k0 + 16)
                    t = P.dma('sp', hb[:, k0:k1, :], src[k0 * 128:k1 * 128, tok].rearrange("(k p) t -> p k t", p=128), sem,
                              deps=deps if k0 == 0 else ())
                return t
            return load

        def linear(st, groups, ncols, NBc, KCin, load_h, epilogue, ntiles=NT, KSUB=8, TW=1, NSETS=2):
            ng = len(groups)
            nm = NBc // 128
            hb = sb("hb", [128, KCin, 512 * TW], BF16, st)
            stg = Ring(P, st, "wst", [128, KSUB, NBc], F32, 3)
            wbf = Ring(P, st, "wbf", [128, KSUB, NBc], BF16, 3, with_sems=False)
            pacc = ps("pacc", [128, NSETS, ng, nm, TW, 512], F32, st)
            pfree = [[] for _ in range(NSETS)]
            hfree = None
            ncb = ncols // NBc
            assert ncols % NBc == 0 and ntiles % TW == 0
            it = 0
            for tp in range(ntiles // TW):
                t_hs = []
                for w in range(TW):
                    t_hs.append(load_h(tp * TW + w, hb[:, :, w * 512:(w + 1) * 512], [hfree]))
                tlast = None
                for cb in range(ncb):
                    pset = it % NSETS
                    it += 1
                    c0 = cb * NBc
                    for g, (W, koff) in enumerate(groups):
                        nk = W.shape[0] // 128
                        for k0 in range(0, nk, KSUB):
                            kn = min(KSUB, nk - k0)
                            ks, sbuf_, sfr = stg.next()
                            t1 = P.dma('sp', sbuf_[:, 0:kn, :],
                                       W[k0 * 128:(k0 + kn) * 128, c0:c0 + NBc].rearrange("(k p) n -> p k n", p=128),
                                       stg.sems[ks], deps=[sfr])
                            kb, wb_, bfr = wbf.next()
                            t2 = P.op('pool', lambda e: e.tensor_copy(wb_[:, 0:kn, :], sbuf_[:, 0:kn, :]), deps=[t1, bfr])
                            stg.free[ks] = t2
                            for m in range(nm):
                                for w in range(TW):
                                    for kk in range(kn):
                                        kc = k0 + kk
                                        last = (m == nm - 1 and w == TW - 1 and kk == kn - 1)
                                        tlast = P.op('pe', lambda e: e.matmul(
                                            pacc[:, pset, g, m, w, :], wb_[:, kk, m * 128:(m + 1) * 128],
                                            hb[:, koff + kc, w * 512:(w + 1) * 512],
                                            start=(kc == 0), stop=(kc == nk - 1)),
                                            deps=[t2] + t_hs + pfree[pset], sig=last)
                            wbf.free[kb] = tlast
                    pf = []
                    for w in range(TW):
                        r_ = epilogue(tp * TW + w, cb, [[pacc[:, pset, g, m, w, :] for m in range(nm)] for g in range(ng)], tlast)
                        pf.extend(r_ if isinstance(r_, list) else [r_])
                    pfree[pset] = pf
                hfree = tlast
            P.barrier()

        def phase_load_x(b):
            with phase() as st:
                xr = Ring(P, st, "xrow", [128, D], F32, 2)
                xo = Ring(P, st, "xo", [128, KC, 128], F32, 2)
                ptp = ps("ptp", [128, 2, 4, 128], F32, st)
                pfree = [None, None]
                cnt = 0
                for tb in range(S // 128):
                    k, rb, fr = xr.next()
                    t1 = P.dma('sp', rb[:], x_in[b, tb * 128:(tb + 1) * 128, :], xr.sems[k], deps=[fr])
                    ko, ob, ofr = xo.next()
                    tl = None
                    lastd = {}
                    for k4 in range(0, KC, 4):
                        j = cnt % 2
                        cnt += 1
                        tp = None
                        for i in range(4):
                            tp = P.op('pe', lambda e: e.transpose(ptp[:, j, i, :], rb[:, (k4 + i) * 128:(k4 + i + 1) * 128], ident),
                                      deps=[t1, t_c, pfree[j]], sig=(i == 3))
                        eng = 'act' if (k4 // 4) % 2 == 0 else 'dve'
                        if eng == 'act':
                            tl = P.op('act', lambda e: e.activation(out=ob[:, k4:k4 + 4, :], in_=ptp[:, j, :, :], func=AF.Copy),
                                      deps=[tp, ofr])
                        else:
                            tl = P.op('dve', lambda e: e.tensor_copy(ob[:, k4:k4 + 4, :], ptp[:, j, :, :]), deps=[tp, ofr])
                        pfree[j] = tl
                        lastd[eng] = tl
                    xr.free[k] = tp
                    t_o = P.dma('pool', xT[:, tb * 128:(tb + 1) * 128].rearrange("(k p) t -> p k t", p=128), ob[:],
                                xo.sems[ko], deps=list(lastd.values()))
                    xo.free[ko] = t_o
                P.barrier()

        def evac_store(st, dst, row0):
            stg = Ring(P, st, "ev", [128, 512], F32, 4)

            def epi(tt, cb, pss, tdep):
                tok = slice(tt * 512, (tt + 1) * 512)
                tl = None
                lastt = {}
                nm = len(pss[0])
                for m, p_ in enumerate(pss[0]):
                    k, sbuf_, fr = stg.next()
                    r0 = row0 + (cb * nm + m) * 128
                    if m % 2 == 0:
                        tl = P.op('act', lambda e: e.activation(out=sbuf_[:], in_=p_, func=AF.Copy), deps=[tdep, fr])
                        q = 'act'
                    else:
                        tl = P.op('dve', lambda e: e.tensor_copy(sbuf_[:], p_), deps=[tdep, fr])
                        q = 'pool'
                    stg.free[k] = P.dma(q, dst[r0:r0 + 128, tok], sbuf_[:], stg.sems[k], deps=[tl])
                    lastt[q] = tl
                return list(lastt.values())
            return epi

        def phase_A(b, l):
            with phase() as st:
                ld = norm_loader(st, lambda kc: modA[:, l, 0, b, kc:kc + 1], mod_shift(l, 0, b))
                linear(st, [(w_in[l, :, :], 0)], INW, 128, KC, ld, evac_store(st, proj, 0), TW=2)

        def phase_pool(b, l):
            PGC = cfg.PG // 128
            with phase() as st:
                u = sb("pu", [128, PGC, S], F32, st)
                ta = sb("pta", [128, S], F32, st)
                tb_ = sb("ptb", [128, S], F32, st)
                inv = sb("pinv", [128, S], F32, st)
                pl = sb("ppl", [128, PGC, S], BF16, st)
                wf = sb("pwf", [128, PGC, cfg.PG], F32, st)
                wbf_ = sb("pwb", [128, PGC, cfg.PG], BF16, st)
                ob = Ring(P, st, "pob", [128, 512], BF16, 2)
                pp = ps("ppp", [128, 2, 512], F32, st)
                pfree = [None, None]
                lsem = P.sem("pl")
                cnt = 0
                for g, w in enumerate(POOL_WINDOWS):
                    t0 = P.dma('sp', u[:], proj[g * cfg.PG:(g + 1) * cfg.PG, :].rearrange("(k p) t -> p k t", p=128), lsem)
                    t0 = P.dma('sp', inv[:], consts[:, cfg.c_inv + g * S: cfg.c_inv + (g + 1) * S], lsem)
                    t0 = P.dma('sp', wf[:], w_pool[l, g, :, :].rearrange("(k p) n -> p k n", p=128), lsem)
                    tw = P.op('pool', lambda e: e.tensor_copy(wbf_[:], wf[:]), deps=[t0])
                    for c_ in range(PGC):
                        src = u[:, c_, :]
                        cur, nxt = ta, tb_
                        sh = 1
                        tl = t0
                        first = True
                        while sh < w:
                            a_in = src if first else cur[:]
                            t1 = P.op('dve', lambda e: e.tensor_tensor(out=nxt[:, sh:], in0=a_in[:, sh:], in1=a_in[:, :S - sh], op=ALU.add),
                                      deps=[tl])
                            tl = P.op('dve', lambda e: e.tensor_copy(nxt[:, 0:sh], a_in[:, 0:sh]), deps=[t1])
                            cur, nxt = nxt, cur
                            sh *= 2
                            first = False
                        t2 = P.op('dve', lambda e: e.tensor_tensor(out=cur[:], in0=cur[:], in1=inv[:], op=ALU.mult), deps=[tl])
                        tl = P.op('dve', lambda e: e.tensor_tensor(out=pl[:, c_, :], in0=cur[:], in1=src, op=ALU.subtract), deps=[t2])
                    for dch in range(PGC):
                        for tt in range(NT):
                            j = cnt % 2
                            cnt += 1
                            tm = None
                            for c_ in range(PGC):
                                tm = P.op('pe', lambda e: e.matmul(pp[:, j, :], wbf_[:, c_, dch * 128:(dch + 1) * 128],
                                                                   pl[:, c_, tt * 512:(tt + 1) * 512], start=(c_ == 0), stop=(c_ == PGC - 1)),
                                          deps=[tw, tl, pfree[j]], sig=(c_ == PGC - 1))
                            k, o_, fr = ob.next()
                            ch = g * PGC + dch
                            te = P.op('act', lambda e: e.activation(out=o_[:], in_=pp[:, j, :], func=AF.Identity,
                                                                    scale=pscale[:, l, ch:ch + 1]), deps=[tm, fr])
                            pfree[j] = te
                            ob.free[k] = P.dma('act', ymix[ch * 128:(ch + 1) * 128, tt * 512:(tt + 1) * 512], o_[:], ob.sems[k], deps=[te])
                    P.barrier()

        def phase_attn(b, l):
            scale = 128 ** -0.5
            NBLK = S // 128
            with phase() as st:
                qf = sb("aqf", [128, S], F32, st)
                kf = sb("akf", [128, S], F32, st)
                vf = sb("avf", [128, S], F32, st)
                qb = sb("aqb", [128, S], BF16, st)
                kb = sb("akb", [128, S], BF16, st)
                vb = sb("avb", [128, S], BF16, st)
                vT = sb("avT", [128, NBLK, 128], BF16, st)
                R = sb("aR", [128, 512], F32, st)
                e_r = Ring(P, st, "ae", [128, 512], F32, 2, with_sems=False)
                sp_r = Ring(P, st, "asp", [128, 512], F32, 2, with_sems=False)
                t1_r = Ring(P, st, "at1", [128, 512], F32, 2, with_sems=False)
                a_r = Ring(P, st, "aa", [128, 512], BF16, 2, with_sems=False)
                ob = Ring(P, st, "aob", [128, 512], BF16, 2)
                pz = ps("apz", [128, 2, 512], F32, st)
                ptr = ps("aptr", [128, 2, 512], F32, st)
                pon = ps("apon", [128, 2, 512], F32, st)
                po = ps("apo", [128, 512], F32, st)
                ptv = ps("aptv", [128, 4, 256], BF16, st)
                lsem = P.sem("al")
                pzf, ptrf, ponf = [None, None], [None, None], [None, None]
                pof = None
                cnt = 0
                hfree = None
                tRlast = None
                for h in range(H):
                    r0 = h * 128
                    P.dma('sp', qf[:], proj[cfg.o_q + r0: cfg.o_q + r0 + 128, :], lsem, deps=[hfree])
                    P.dma('sp', kf[:], proj[cfg.o_k + r0: cfg.o_k + r0 + 128, :], lsem)
                    t0 = P.dma('sp', vf[:], proj[cfg.o_v + r0: cfg.o_v + r0 + 128, :], lsem)
                    tq = P.op('pool', lambda e: e.tensor_copy(qb[:], qf[:]), deps=[t0])
                    tk = P.op('pool', lambda e: e.tensor_copy(kb[:], kf[:]), deps=[tq])
                    tv = P.op('dve', lambda e: e.tensor_copy(vb[:], vf[:]), deps=[t0])
                    tvT = None
                    for b4 in range(0, NBLK, 4):
                        tp = None
                        for i in range(4):
                            tp = P.op('pe', lambda e: e.transpose(ptv[:, i, 0:128], vb[:, (b4 + i) * 128:(b4 + i + 1) * 128], identb[:]),
                                      deps=[tv, t_ib, tvT], sig=(i == 3))
                        tvT = P.op('act', lambda e: e.activation(out=vT[:, b4:b4 + 4, :], in_=ptv[:, :, 0:128], func=AF.Copy), deps=[tp])
                    for TB in range(S // 512):
                        qs = slice(TB * 512, (TB + 1) * 512)
                        nkb = 4 * TB + 4
                        tR = P.op('pool', lambda e: e.memset(R[:], 0.0), deps=[hfree, tRlast])
                        tav = None
                        for idx, sbk in enumerate(range(nkb - 1, -1, -1)):
                            j = cnt % 2
                            cnt += 1
                            diag = sbk - 4 * TB
                            tz = P.op('pe', lambda e: e.matmul(pz[:, j, :], kb[:, sbk * 128:(sbk + 1) * 128], qb[:, qs], start=True, stop=True),
                                      deps=[tq, tk, pzf[j]])
                            _, e_, _ = e_r.next()
                            _, sp_, _ = sp_r.next()
                            _, t1_, _ = t1_r.next()
                            _, a_, _ = a_r.next()
                            te = P.op('act', lambda e: e.activation(out=e_[:], in_=pz[:, j, :], func=AF.Exp, scale=scale), deps=[tz, tav])
                            ts = P.op('act', lambda e: e.activation(out=sp_[:], in_=e_[:], func=AF.Ln, bias=1.0, scale=1.0), deps=[te])
                            if diag >= 0:
                                ts = P.op('dve', lambda e: e.tensor_tensor(out=sp_[:], in0=sp_[:], in1=attm[diag], op=ALU.mult), deps=[ts, t_c])
                            ttr = P.op('pe', lambda e: e.matmul(ptr[:, j, :], tri, sp_[:], start=True, stop=True), deps=[ts, ptrf[j]])
                            ton = P.op('pe', lambda e: e.matmul(pon[:, j, :], ones, sp_[:], start=True, stop=True), deps=[ts, ponf[j]])
                            t1 = P.op('dve', lambda e: e.scalar_tensor_tensor(out=t1_[:], in0=pz[:, j, :], scalar=scale, in1=sp_[:],
                                                                              op0=ALU.mult, op1=ALU.subtract), deps=[ts])
                            pzf[j] = t1
                            t2 = P.op('dve', lambda e: e.tensor_tensor(out=t1_[:], in0=t1_[:], in1=ptr[:, j, :], op=ALU.subtract), deps=[t1, ttr])
                            ptrf[j] = t2
                            t3 = P.op('dve', lambda e: e.tensor_tensor(out=t1_[:], in0=t1_[:], in1=R[:], op=ALU.subtract), deps=[t2, tR])
                            tR = P.op('dve', lambda e: e.tensor_tensor(out=R[:], in0=R[:], in1=pon[:, j, :], op=ALU.add), deps=[t3, ton])
                            ponf[j] = tR
                            ta_ = P.op('act', lambda e: e.activation(out=a_[:], in_=t1_[:], func=AF.Exp), deps=[t3])
                            if diag >= 0:
                                ta_ = P.op('pool', lambda e: e.tensor_tensor(out=a_[:], in0=a_[:], in1=attm[diag], op=ALU.mult), deps=[ta_, t_c])
                            tav = P.op('pe', lambda e: e.matmul(po[:], vT[:, sbk, :], a_[:], start=(idx == 0), stop=(idx == nkb - 1)),
                                       deps=[ta_, tvT, pof])
                        k, o_, fr = ob.next()
                        te2 = P.op('act', lambda e: e.activation(out=o_[:], in_=po[:], func=AF.Copy), deps=[tav, fr])
                        pof = te2
                        ch = PW // 128 + h
                        ob.free[k] = P.dma('act', ymix[ch * 128:(ch + 1) * 128, qs], o_[:], ob.sems[k], deps=[te2])
                        hfree = tav
                        tRlast = tR
                P.barrier()

        def phase_hgrn(b, l):
            NBLK = S // 128
            NCH = S // 64
            with phase() as st:
                qf = sb("hqf", [128, S], F32, st)
                ff = sb("hff", [128, S], F32, st)
                i_f = sb("hif", [128, S], F32, st)
                gf = sb("hgf", [128, S], F32, st)
                scm = sb("hscm", [128, S], F32, st)
                bb = sb("hbb", [128, S], F32, st)
                eb = sb("heb", [128, S], F32, st)
                tmp = sb("htmp", [128, S], F32, st)
                Qt = sb("hQt", [128, S], BF16, st)
                Kt = sb("hKt", [128, S], BF16, st)
                ib = sb("hib", [128, S], BF16, st)
                KtT = sb("hKtT", [128, NBLK, 128], BF16, st)
                iT = sb("hiT", [128, NBLK, 128], BF16, st)
                o_f = sb("hof", [128, S], F32, st)
                Sf = sb("hSf", [128, 128], F32, st)
                bmid = sb("hbmid", [128, NCH, 1], F32, st)
                ebm = sb("hebm", [128, NCH, 1], F32, st)
                Sin = Ring(P, st, "hSin", [128, 128], F32, 2, with_sems=False)
                Sb = Ring(P, st, "hSb", [128, 128], BF16, 3, with_sems=False)
                scb = Ring(P, st, "hscb", [128, 128], BF16, 2, with_sems=False)
                ob = Ring(P, st, "hob", [128, 512], BF16, 2)
                rs = sb("hrs", [128, 512], F32, st)
                psc = ps("hpsc", [128, 2, 512], F32, st)
                pou = ps("hpou", [128, 2, 512], F32, st)
                pst_ = ps("hpst", [128, 2, 512], F32, st)
                pnr = ps("hpnr", [128, 512], F32, st)
                pt = ps("hpt", [128, 4, 256], BF16, st)
                lsem = P.sem("hl")
                t_m = P.dma('sp', scm[:], consts[:, cfg.c_scan:cfg.c_scan + S], lsem)
                tz0 = None
                for bf_ in scb.bufs:
                    tz0 = P.op('dve', lambda e: e.memset(bf_[:], 0.0), deps=[tz0])
                hfree = None
                pscf, pouf, pstf = [None, None], [None, None], [None, None]
                pnrf = None
                cnt = 0
                for h in range(H):
                    r0 = h * 128
                    P.dma('sp', qf[:], proj[cfg.o_qh + r0: cfg.o_qh + r0 + 128, :], lsem, deps=[hfree])
                    P.dma('sp', ff[:], proj[cfg.o_fh + r0: cfg.o_fh + r0 + 128, :], lsem)
                    P.dma('sp', i_f[:], proj[cfg.o_ih + r0: cfg.o_ih + r0 + 128, :], lsem)
                    t0 = P.dma('sp', gf[:], proj[cfg.o_gh + r0: cfg.o_gh + r0 + 128, :], lsem)
                    t1 = P.op('act', lambda e: e.activation(out=ff[:], in_=ff[:], func=AF.Sigmoid), deps=[t0])
                    t1 = P.op('dve', lambda e: e.tensor_scalar(out=ff[:], in0=ff[:], scalar1=oml[:, l, h:h + 1], scalar2=lbt[:, l, h:h + 1],
                                                               op0=ALU.mult, op1=ALU.add), deps=[t1])
                    t2 = P.op('act', lambda e: e.activation(out=tmp[:], in_=ff[:], func=AF.Ln), deps=[t1])
                    t3 = P.op('dve', lambda e: e.tensor_tensor_scan(out=bb[:], data0=scm[:], data1=tmp[:], initial=0.0,
                                                                    op0=ALU.mult, op1=ALU.add), deps=[t2, t_m])
                    bb3 = bb[:].rearrange("p (c t) -> p c t", t=64)
                    t3 = P.op('dve', lambda e: e.tensor_copy(bmid[:], bb3[:, :, 31:32]), deps=[t3])
                    t3b = P.op('act', lambda e: e.activation(out=ebm[:], in_=bmid[:], func=AF.Exp), deps=[t3])
                    t3 = P.op('dve', lambda e: e.tensor_tensor(out=bb3, in0=bb3, in1=bmid[:].to_broadcast([128, NCH, 64]), op=ALU.subtract), deps=[t3])
                    t4 = P.op('act', lambda e: e.activation(out=eb[:], in_=bb[:], func=AF.Exp), deps=[t3, t3b])
                    t5 = P.op('act', lambda e: e.activation(out=tmp[:], in_=bb[:], func=AF.Exp, scale=-1.0), deps=[t3])
                    t6 = P.op('dve', lambda e: e.tensor_scalar(out=ff[:], in0=ff[:], scalar1=-1.0, scalar2=1.0, op0=ALU.mult, op1=ALU.add),
                              deps=[t2])
                    tK = P.op('dve', lambda e: e.tensor_tensor(out=Kt[:], in0=ff[:], in1=tmp[:], op=ALU.mult), deps=[t5, t6])
                    t7 = P.op('act', lambda e: e.activation(out=qf[:], in_=qf[:], func=AF.Silu), deps=[t0])
                    tQ = P.op('dve', lambda e: e.tensor_tensor(out=Qt[:], in0=qf[:], in1=eb[:], op=ALU.mult), deps=[t7, t4])
                    ti = P.op('pool', lambda e: e.tensor_copy(ib[:], i_f[:]), deps=[t0])
                    tT = None
                    for (src, dst, tsrc) in ((Kt, KtT, tK), (ib, iT, ti)):
                        for b4 in range(0, NBLK, 4):
                            tp = None
                            for i in range(4):
                                tp = P.op('pe', lambda e: e.transpose(pt[:, i, 0:128], src[:, (b4 + i) * 128:(b4 + i + 1) * 128], identb[:]),
                                          deps=[tsrc, t_ib, tT], sig=(i == 3))
                            tT = P.op('act', lambda e: e.activation(out=dst[:, b4:b4 + 4, :], in_=pt[:, :, 0:128], func=AF.Copy), deps=[tp])
                    tS = P.op('dve', lambda e: e.memset(Sf[:], 0.0), deps=[hfree])
                    to_all = None
                    tSb = None
                    for blk in range(NBLK):
                        j = cnt % 2
                        cnt += 1
                        bs = slice(blk * 128, (blk + 1) * 128)
                        tsc = P.op('pe', lambda e: e.matmul(psc[:, j, 0:128], Kt[:, bs], Qt[:, bs], start=True, stop=True), deps=[tK, tQ, pscf[j]])
                        _, sc_, _ = scb.next()
                        tm_ = P.op('dve', lambda e: e.copy_predicated(out=sc_[:], mask=hgmi[:], data=psc[:, j, 0:128]), deps=[tsc, t_hi, tz0])
                        pscf[j] = tm_
                        tin = P.op('pe', lambda e: e.matmul(pou[:, j, 0:128], iT[:, blk, :], sc_[:], start=True, stop=False), deps=[tm_, tT, pouf[j]])
                        for hf in range(2):
                            c_ = 2 * blk + hf
                            cs = slice(c_ * 64, (c_ + 1) * 64)
                            pr = slice(hf * 64, (hf + 1) * 64)
                            _, sin_, _ = Sin.next()
                            tSi = P.op('dve', lambda e: e.tensor_scalar(out=sin_[:], in0=Sf[:], scalar1=ebm[:, c_, :], scalar2=None, op0=ALU.mult),
                                       deps=[tS, t3b, tSb])
                            _, sbcur, _ = Sb.next()
                            tSb = P.op('pool', lambda e: e.tensor_copy(sbcur[:], sin_[:]), deps=[tSi, tin])
                            tin = P.op('pe', lambda e: e.matmul(pou[:, j, hf * 64:(hf + 1) * 64], sbcur[:], Qt[:, cs], start=False, stop=(hf == 1)),
                                       deps=[tSb, tin])
                            tpm = P.op('pe', lambda e: e.matmul(pst_[:, hf, 0:128], KtT[pr, blk, :], iT[pr, blk, :], start=True, stop=True),
                                       deps=[tT, pstf[hf]])
                            tS = P.op('dve', lambda e: e.tensor_tensor(out=Sf[:], in0=sin_[:], in1=pst_[:, hf, 0:128], op=ALU.add), deps=[tpm, tSi])
                            pstf[hf] = tS
                            tS = P.op('dve', lambda e: e.tensor_scalar(out=Sf[:], in0=Sf[:], scalar1=eb[:, c_ * 64 + 63:c_ * 64 + 64], scalar2=None,
                                                                       op0=ALU.mult), deps=[tS, t4])
                        to_all = P.op('act', lambda e: e.activation(out=o_f[:, bs], in_=pou[:, j, 0:128], func=AF.Copy), deps=[tin])
                        pouf[j] = to_all
                    tg = P.op('act', lambda e: e.activation(out=gf[:], in_=gf[:], func=AF.Silu), deps=[t0])
                    tg = P.op('dve', lambda e: e.tensor_scalar(out=gf[:], in0=gf[:], scalar1=hgn[:, l:l + 1], scalar2=None, op0=ALU.mult), deps=[tg])
                    for tt in range(NT):
                        ts_ = slice(tt * 512, (tt + 1) * 512)
                        tq2 = P.op('act', lambda e: e.activation(out=tmp[:, ts_], in_=o_f[:, ts_], func=AF.Square), deps=[to_all, tK])
                        tn = P.op('pe', lambda e: e.matmul(pnr[:], ones, tmp[:, ts_], start=True, stop=True), deps=[tq2, pnrf])
                        tr = P.op('dve', lambda e: e.tensor_scalar(out=rs[:], in0=pnr[:], scalar1=1.0 / 128, scalar2=EPS, op0=ALU.mult, op1=ALU.add), deps=[tn])
                        pnrf = tr
                        tr = P.op('act', lambda e: e.activation(out=rs[:], in_=rs[:], func=AF.Sqrt), deps=[tr])
                        tr = P.op('dve', lambda e: e.reciprocal(rs[:], rs[:]), deps=[tr])
                        tr = P.op('dve', lambda e: e.tensor_tensor(out=rs[:], in0=rs[:], in1=o_f[:, ts_], op=ALU.mult), deps=[tr])
                        k, o_, fr = ob.next()
                        tr = P.op('dve', lambda e: e.tensor_tensor(out=o_[:], in0=rs[:], in1=gf[:, ts_], op=ALU.mult), deps=[tr, tg, fr])
                        ch = (PW + SBW) // 128 + h
                        ob.free[k] = P.dma('sp', ymix[ch * 128:(ch + 1) * 128, ts_], o_[:], ob.sems[k], deps=[tr])
                        hfree = tr
                P.barrier()

        def phase_C(b, l):
            with phase() as st:
                gl = Ring(P, st, "cgl", [128, 3, 512], F32, 2)
                acc = Ring(P, st, "cacc", [128, 512], F32, 2, with_sems=False)
                tm_ = Ring(P, st, "ctm", [128, 512], F32, 2, with_sems=False)
                ob = Ring(P, st, "cob", [128, 512], BF16, 2)

                def epi(tt, cb, pss, tdep):
                    tok = slice(tt * 512, (tt + 1) * 512)
                    m = cb
                    k, g_, fr = gl.next()
                    src = proj[cfg.o_gate:cfg.o_gate + 3 * D, tok].rearrange("(r d) t -> d r t", r=3)[m * 128:(m + 1) * 128]
                    t0 = P.dma('pool', g_[:], src, gl.sems[k], deps=[fr])
                    ts = P.op('act', lambda e: e.activation(out=g_[:], in_=g_[:], func=AF.Sigmoid), deps=[t0])
                    _, a_, _ = acc.next()
                    _, t_, _ = tm_.next()
                    ko, o_, ofr = ob.next()
                    t1 = P.op('dve', lambda e: e.tensor_tensor(out=a_[:], in0=g_[:, 0, :], in1=pss[0][0], op=ALU.mult), deps=[ts, tdep])
                    t2 = P.op('dve', lambda e: e.tensor_tensor(out=t_[:], in0=g_[:, 1, :], in1=pss[1][0], op=ALU.mult), deps=[t1])
                    t2 = P.op('dve', lambda e: e.tensor_tensor(out=a_[:], in0=a_[:], in1=t_[:], op=ALU.add), deps=[t2])
                    t3 = P.op('dve', lambda e: e.tensor_tensor(out=t_[:], in0=g_[:, 2, :], in1=pss[2][0], op=ALU.mult), deps=[t2])
                    t3 = P.op('dve', lambda e: e.tensor_tensor(out=o_[:], in0=a_[:], in1=t_[:], op=ALU.add), deps=[t3, ofr])
                    gl.free[k] = t3
                    ob.free[ko] = P.dma('act', merged[m * 128:(m + 1) * 128, tok], o_[:], ob.sems[ko], deps=[t3])
                    return t3
                groups = [(w_br_pool[l, :, :], 0), (w_br_sb[l, :, :], PW // 128), (w_br_hg[l, :, :], (PW + SBW) // 128)]
                linear(st, groups, D, 128, KC, dram_loader(st, ymix, KC), epi)

        def resid_epi(st, gate):
            xr = Ring(P, st, "rx", [128, 512], F32, 3)

            def epi(tt, cb, pss, tdep):
                tok = slice(tt * 512, (tt + 1) * 512)
                tl = None
                nm = len(pss[0])
                for m, p_ in enumerate(pss[0]):
                    ch = cb * nm + m
                    k, x_, fr = xr.next()
                    t0 = P.dma('pool', x_[:], xT[ch * 128:(ch + 1) * 128, tok], xr.sems[k], deps=[fr])
                    tl = P.op('dve', lambda e: e.scalar_tensor_tensor(out=x_[:], in0=p_, scalar=gate(ch), in1=x_[:],
                                                                      op0=ALU.mult, op1=ALU.add), deps=[t0, tdep])
                    xr.free[k] = P.dma('act', xT[ch * 128:(ch + 1) * 128, tok], x_[:], xr.sems[k], deps=[tl])
                return tl
            return epi

        def phase_D(b, l):
            with phase() as st:
                linear(st, [(w_out[l, :, :], 0)], D, 256, KC, dram_loader(st, merged, KC), resid_epi(st, mod_gate(l, 0, b)), TW=2)

        def ffn_up_epi(st):
            sa = Ring(P, st, "fsa", [128, 512], F32, 2, with_sems=False)
            ob = Ring(P, st, "fob", [128, 512], BF16, 3)

            def epi(tt, cb, pss, tdep, row0=0, bc=None):
                tok = slice(tt * 512, (tt + 1) * 512)
                tl = None
                nm = len(pss[0])
                for m in range(nm):
                    _, s_, _ = sa.next()
                    k, o_, fr = ob.next()
                    t1 = P.op('act', lambda e: e.activation(out=s_[:], in_=pss[0][m], func=AF.Silu), deps=[tdep, tl])
                    if bc is None:
                        tl = P.op('dve', lambda e: e.tensor_tensor(out=o_[:], in0=s_[:], in1=pss[1][m], op=ALU.mult), deps=[t1, fr])
                    else:
                        t2 = P.op('dve', lambda e: e.tensor_tensor(out=s_[:], in0=s_[:], in1=pss[1][m], op=ALU.mult), deps=[t1])
                        tl = P.op('dve', lambda e: e.tensor_tensor(out=o_[:], in0=s_[:], in1=bc, op=ALU.mult), deps=[t2, fr])
                    r0 = row0 + (cb * nm + m) * 128
                    ob.free[k] = P.dma('act', hid[r0:r0 + 128, tok], o_[:], ob.sems[k], deps=[tl])
                return tl
            return epi

        def phase_ffn_dense(b, l):
            j = l // 2
            with phase() as st:
                ld = norm_loader(st, lambda kc: modA[:, l, 1, b, kc:kc + 1], mod_shift(l, 1, b))
                linear(st, [(ffn_w1[j, :, :], 0), (ffn_w3[j, :, :], 0)], cfg.DFF, 128, KC, ld, ffn_up_epi(st), TW=2, NSETS=1)
            with phase() as st:
                linear(st, [(ffn_w2[j, :, :], 0)], D, 256, cfg.DFF // 128, dram_loader(st, hid, cfg.DFF // 128),
                       resid_epi(st, mod_gate(l, 1, b)))

        def phase_ffn_moe(b, l):
            j = l // 2
            NE, DE = cfg.NE, cfg.DE
            with phase() as st:
                hb = sb("mhb", [128, KC, 512], BF16, st)
                wr = sb("mwr", [128, KC, NE], F32, st)
                brt = sb("mbr", [NE, 1], F32, st)
                lg = sb("mlg", [NE, 512], F32, st)
                lgT = sb("mlgT", [128, 4, NE], F32, st)
                wk = sb("mwk", [128, 4, NE], F32, st)
                mx = sb("mmx", [128, 4, 1], F32, st)
                mx2 = sb("mmx2", [128, 4, 1], F32, st)
                cmb = sb("mcmb", [128, 4, NE], F32, st)
                cmT = sb("mcmT", [NE, 512], F32, st)
                bcs = sb("mbcs", [128, NE, 512], F32, st)
                sel = sb("msel", [NE, NE, 128], F32, st)
                plg = ps("mplg", [128, 512], F32, st)
                plt = ps("mplt", [128, 4, 128], F32, st)
                msem = P.sem("ml")
                P.dma('sp', wr[:], w_router[j, :, :].rearrange("(k p) n -> p k n", p=128), msem)
                t0 = P.dma('sp', brt[:], b_router[j, :].rearrange("(n o) -> n o", o=1), msem)
                tsel = t0
                tsel = P.op('dve', lambda e: e.memset(sel[:], 0.0), deps=[t0])
                for e_ in range(NE):
                    tsel = P.op('dve', lambda e: e.tensor_scalar(out=sel[:, e_, :], in0=sel[:, e_, :], scalar1=ident[0:NE, e_:e_ + 1],
                                                                 scalar2=None, op0=ALU.add), deps=[tsel, t_c])
                state = {'tok': None}

                def router(tt, xt, tdep):
                    tm = None
                    for kc in range(KC):
                        tm = P.op('pe', lambda e: e.matmul(plg[0:NE, :], wr[:, kc, :], xt[:, kc, :], start=(kc == 0), stop=(kc == KC - 1)),
                                  deps=[tdep, t0, state['tok']], sig=(kc == KC - 1))
                    t1 = P.op('act', lambda e: e.activation(out=lg[:], in_=plg[0:NE, :], func=AF.Identity, bias=brt[:, 0:1], scale=1.0), deps=[tm])
                    tp = None
                    for i in range(4):
                        tp = P.op('pe', lambda e: e.transpose(plt[:, i, 0:NE], lg[:, i * 128:(i + 1) * 128], ident[0:NE, 0:NE]), deps=[t1, t_c], sig=(i == 3))
                    t2 = P.op('dve', lambda e: e.tensor_copy(lgT[:], plt[:, :, 0:NE]), deps=[tp])
                    t3 = t2
                    for i in range(4):
                        L_ = lgT[:, i, :]
                        W_ = wk[:, i, :]
                        C_ = cmb[:, i, :]
                        m1 = mx[:, i, :]
                        m2 = mx2[:, i, :]
                        t3 = P.op('dve', lambda e: e.reduce_max(out=m1, in_=L_, axis=mybir.AxisListType.X), deps=[t3])
                        t3 = P.op('dve', lambda e: e.tensor_scalar(out=W_, in0=L_, scalar1=m1, scalar2=None, op0=ALU.is_ge), deps=[t3])
                        t3 = P.op('dve', lambda e: e.scalar_tensor_tensor(out=W_, in0=W_, scalar=-1e30, in1=L_, op0=ALU.mult, op1=ALU.add), deps=[t3])
                        t3 = P.op('dve', lambda e: e.reduce_max(out=m2, in_=W_, axis=mybir.AxisListType.X), deps=[t3])
                        t3 = P.op('dve', lambda e: e.tensor_scalar(out=W_, in0=L_, scalar1=m2, scalar2=None, op0=ALU.is_ge), deps=[t3])
                        t3 = P.op('dve', lambda e: e.tensor_scalar(out=C_, in0=L_, scalar1=m1, scalar2=None, op0=ALU.subtract), deps=[t3])
                        t3 = P.op('act', lambda e: e.activation(out=C_, in_=C_, func=AF.Exp), deps=[t3])
                        t3 = P.op('dve', lambda e: e.tensor_tensor(out=C_, in0=C_, in1=W_, op=ALU.mult), deps=[t3])
                        t3 = P.op('dve', lambda e: e.reduce_sum(out=m2, in_=C_, axis=mybir.AxisListType.X), deps=[t3])
                        t3 = P.op('dve', lambda e: e.reciprocal(m2, m2), deps=[t3])
                        t3 = P.op('dve', lambda e: e.tensor_scalar(out=C_, in0=C_, scalar1=m2, scalar2=None, op0=ALU.mult), deps=[t3])
                    tp = None
                    for i in range(4):
                        tp = P.op('pe', lambda e: e.transpose(plg[0:NE, i * 128:(i + 1) * 128], cmb[:, i, :], ident), deps=[t3, t1], sig=(i == 3))
                    t4 = P.op('act', lambda e: e.activation(out=cmT[:], in_=plg[0:NE, :], func=AF.Copy), deps=[tp])
                    tb_ = t4
                    for e_ in range(NE):
                        tq = P.op('pe', lambda e: e.matmul(plg[:], sel[:, e_, :], cmT[:], start=True, stop=True), deps=[t4, tsel, tb_])
                        tb_ = P.op('dve', lambda e: e.tensor_copy(bcs[:, e_, :], plg[:]), deps=[tq])
                    state['tok'] = tb_
                    return tb_

                ld = norm_loader(st, lambda kc: modA[:, l, 1, b, kc:kc + 1], mod_shift(l, 1, b), router=router)
                moe_linear_up(st, b, l, j, ld, bcs, state)
            SEG = 2
            for s0 in range(0, NE, SEG):
                with phase() as st:
                    KCs = SEG * DE // 128
                    groups = [(moe_w2[j, :, :, :].rearrange("e k n -> (e k) n")[s0 * DE:(s0 + SEG) * DE, :], 0)]

                    def ldr(st_):
                        sem = P.sem("hl2")

                        def load(tt, hb, deps):
                            tok = slice(tt * 512, (tt + 1) * 512)
                            t = None
                            for k0 in range(0, KCs, 16):
                                k1 = min(KCs, k0 + 16)
                                t = P.dma('sp', hb[:, k0:k1, :], hid[s0 * DE + k0 * 128:s0 * DE + k1 * 128, tok].rearrange("(k p) t -> p k t", p=128),
                                          sem, deps=deps if k0 == 0 else ())
                            return t
                        return load
                    linear(st, groups, D, 256, KCs, ldr(st), resid_epi(st, mod_gate(l, 1, b)))

        def moe_linear_up(st, b, l, j, ld, bcs, state):
            NE, DE = cfg.NE, cfg.DE
            KSUB, NBc = 8, 128
            nm = 1
            hb = sb("hb", [128, KC, 512], BF16, st)
            stg = Ring(P, st, "wst", [128, KSUB, NBc], F32, 3)
            wbf = Ring(P, st, "wbf", [128, KSUB, NBc], BF16, 3, with_sems=False)
            pacc = ps("pacc", [128, 2, 2, nm, 512], F32, st)
            pfree = [None, None]
            hfree = None
            cnt = 0
            epi_up = ffn_up_epi(st)
            for tt in range(NT):
                t_h = ld(tt, hb, [hfree])
                t_r = state['tok']
                tlast = None
                for e_ in range(NE):
                    for cb in range(DE // NBc):
                        pset = cnt % 2
                        cnt += 1
                        c0 = cb * NBc
                        for g, W in enumerate((moe_w1[j, e_, :, :], moe_w3[j, e_, :, :])):
                            for k0 in range(0, KC, KSUB):
                                kn = min(KSUB, KC - k0)
                                ks, sbuf_, sfr = stg.next()
                                t1 = P.dma('sp', sbuf_[:, 0:kn, :], W[k0 * 128:(k0 + kn) * 128, c0:c0 + NBc].rearrange("(k p) n -> p k n", p=128),
                                           stg.sems[ks], deps=[sfr])
                                kb, wb_, bfr = wbf.next()
                                t2 = P.op('pool', lambda e: e.tensor_copy(wb_[:, 0:kn, :], sbuf_[:, 0:kn, :]), deps=[t1, bfr])
                                stg.free[ks] = t2
                                for m in range(nm):
                                    for kk in range(kn):
                                        kc = k0 + kk
                                        last = (m == nm - 1 and kk == kn - 1)
                                        tlast = P.op('pe', lambda e: e.matmul(pacc[:, pset, g, m, :], wb_[:, kk, m * 128:(m + 1) * 128], hb[:, kc, :],
                                                                              start=(kc == 0), stop=(kc == KC - 1)),
                                                     deps=[t2, t_h, pfree[pset]], sig=last)
                                wbf.free[kb] = tlast
                        P.wait('dve', t_r)
                        pfree[pset] = epi_up(tt, cb, [[pacc[:, pset, g, m, :] for m in range(nm)] for g in range(2)], tlast,
                                             row0=e_ * DE, bc=bcs[:, e_, :])
                hfree = tlast
                state['tok'] = tlast
            P.barrier()

        def phase_final(b):
            with phase() as st:
                hbuf = None
                xt = sb("fxt", [128, KC, 512], F32, st)
                sqr = Ring(P, st, "fsq", [128, 512], F32, 2, with_sems=False)
                rstd = sb("frs", [128, 512], F32, st)
                psn = ps("fpsn", [128, 512], F32, st)
                ptp = ps("fptp", [128, 2, 512], F32, st)
                orow = Ring(P, st, "forow", [128, D], F32, 2)
                xsem = P.sem("fx")
                xfree = None
                pfree = [None, None]
                cnt = 0
                for tt in range(NT):
                    tok = slice(tt * 512, (tt + 1) * 512)
                    t_ld = None
                    for k0 in range(0, KC, 16):
                        k1 = min(KC, k0 + 16)
                        t_ld = P.dma('sp', xt[:, k0:k1, :], xT[k0 * 128:k1 * 128, tok].rearrange("(k p) t -> p k t", p=128), xsem,
                                     deps=[xfree] if k0 == 0 else ())
                    tm = None
                    for kc in range(KC):
                        k, sq, fr = sqr.next()
                        t1 = P.op('act', lambda e: e.activation(out=sq[:], in_=xt[:, kc, :], func=AF.Square), deps=[t_ld, fr])
                        tm = P.op('pe', lambda e: e.matmul(psn[:], ones, sq[:], start=(kc == 0), stop=(kc == KC - 1)), deps=[t1, t_c])
                        sqr.free[k] = tm
                    t2 = P.op('dve', lambda e: e.tensor_scalar(out=rstd[:], in0=psn[:], scalar1=1.0 / D, scalar2=EPS, op0=ALU.mult, op1=ALU.add), deps=[tm])
                    t2 = P.op('act', lambda e: e.activation(out=rstd[:], in_=rstd[:], func=AF.Sqrt), deps=[t2])
                    t2 = P.op('dve', lambda e: e.reciprocal(rstd[:], rstd[:]), deps=[t2])
                    tl = []
                    for kc in range(KC):
                        t3 = P.op('dve', lambda e: e.scalar_tensor_tensor(out=xt[:, kc, :], in0=xt[:, kc, :], scalar=gfin[:, kc:kc + 1], in1=rstd[:],
                                                                          op0=ALU.mult, op1=ALU.mult), deps=[t2])
                        tl.append(t3)
                    for tb in range(4):
                        ko, ob_, ofr = orow.next()
                        te = None
                        for k4 in range(0, KC, 4):
                            j = cnt % 2
                            cnt += 1
                            tp = None
                            for i in range(4):
                                tp = P.op('pe', lambda e: e.transpose(ptp[:, j, i * 128:(i + 1) * 128], xt[:, k4 + i, tb * 128:(tb + 1) * 128], ident),
                                          deps=[tl[k4 + i], pfree[j]], sig=(i == 3))
                            if (k4 // 4) % 2 == 0:
                                te = P.op('act', lambda e: e.activation(out=ob_[:, k4 * 128:(k4 + 4) * 128], in_=ptp[:, j, :], func=AF.Copy), deps=[tp, ofr])
                                te_a = te
                            else:
                                te = P.op('dve', lambda e: e.tensor_copy(ob_[:, k4 * 128:(k4 + 4) * 128], ptp[:, j, :]), deps=[tp, ofr])
                                te_d = te
                            pfree[j] = te
                        deps_ = [te_a] + ([te_d] if KC > 4 else [])
                        orow.free[ko] = P.dma('pool', out[b, tt * 512 + tb * 128: tt * 512 + (tb + 1) * 128, :], ob_[:], orow.sems[ko], deps=deps_)
                        xfree = tp
                P.barrier()

        stop_after = getattr(cfg, 'stop_after', None)
        for b in range(NB):
            phase_load_x(b)
            for l in range(L):
                phase_A(b, l)
                if stop_after == 'A':
                    break
                phase_pool(b, l)
                phase_attn(b, l)
                phase_hgrn(b, l)
                if stop_after == 'B':
                    break
                phase_C(b, l)
                phase_D(b, l)
                if stop_after == 'D':
                    break
                if l % 2 == 0:
                    phase_ffn_dense(b, l)
                else:
                    phase_ffn_moe(b, l)
                if stop_after == 'F0':
                    break
            phase_final(b)
        P.barrier()
    return nc


FULL = Cfg(NB=1)
NCORES = 8 // FULL.NB


def kernel(**inputs):
    cfg = FULL
    nc = build(cfg)
    cst = make_consts(cfg)
    in_maps = []
    for ci in range(NCORES):
        m = {}
        for k, v in inputs.items():
            v = np.asarray(v)
            if k == 'x':
                m[k] = np.ascontiguousarray(v[ci * cfg.NB:(ci + 1) * cfg.NB])
            elif k == 'c':
                m[k] = np.ascontiguousarray(v[ci * cfg.NB:(ci + 1) * cfg.NB])
            else:
                m[k] = v
        m['consts'] = cst
        in_maps.append(m)
    res = run_bass_kernel_spmd(nc, in_maps, core_ids=list(range(NCORES)))
    outs = [res.results[ci]["out"] for ci in range(NCORES)]
    return np.concatenate(outs, axis=0).astype(np.float32, copy=False)
```

```python
import numpy as np
from contextlib import ExitStack
import concourse.bass as bass
import concourse.mybir as mybir
from concourse.bass_utils import run_bass_kernel_spmd

F32 = mybir.dt.float32
BF16 = mybir.dt.bfloat16
AF = mybir.ActivationFunctionType
ALU = mybir.AluOpType
EPS = 1e-6
POOL_WINDOWS = (2, 4, 8, 16)


class Cfg:
    def __init__(s, D=4096, S=2048, NB=2, DFF=11008, NE=8, DE=3072, L=2, debug=False):
        s.D, s.S, s.NB, s.DFF, s.NE, s.DE, s.L, s.debug = D, S, NB, DFF, NE, DE, L, debug
        s.KC = D // 128
        s.PG = D // 16
        s.PW = 4 * s.PG
        s.SBW = 3 * D // 8
        s.H = s.SBW // 128
        s.HGW = s.SBW
        s.INW = s.PW + 3 * s.SBW + 4 * s.HGW + 3 * D
        s.o_q = s.PW
        s.o_k = s.o_q + s.SBW
        s.o_v = s.o_k + s.SBW
        s.o_qh = s.o_v + s.SBW
        s.o_fh = s.o_qh + s.HGW
        s.o_ih = s.o_fh + s.HGW
        s.o_gh = s.o_ih + s.HGW
        s.o_gate = s.o_gh + s.HGW
        s.NT = S // 512
        s.ND = (L + 1) // 2
        s.NM = L // 2
        s.HID = max(DFF, NE * DE)
        s.c_ident = 0
        s.c_tri = 128
        s.c_ones = 256
        s.c_hgm = 384
        s.c_att = 512
        s.c_scan = 512 + 2048
        s.c_inv = s.c_scan + S
        s.NCONST = s.c_inv + 4 * S


def make_consts(cfg):
    S = cfg.S
    c = np.zeros((128, cfg.NCONST), np.float32)
    p = np.arange(128)[:, None]
    j = np.arange(128)[None, :]
    c[:, 0:128] = (p == j)
    c[:, 128:256] = (p > j)
    c[:, 256:384] = 1.0
    c[:, 384:512] = ((p // 64) == (j // 64)) & (p <= j)
    jj = np.arange(512)[None, :]
    for d in range(4):
        c[:, 512 + d * 512: 512 + (d + 1) * 512] = (jj > d * 128 + p)
    t = np.arange(S)
    c[:, cfg.c_scan:cfg.c_scan + S] = (t % 64 != 0)[None, :]
    for g, w in enumerate(POOL_WINDOWS):
        c[:, cfg.c_inv + g * S: cfg.c_inv + (g + 1) * S] = (1.0 / np.minimum(t + 1, w))[None, :]
    return c


class Sem:
    def __init__(s, h):
        s.h = h
        s.n = 0


class Prog:
    def __init__(s, nc, es):
        s.nc = nc
        s.es = es
        s.engs = {'pe': nc.tensor, 'act': nc.scalar, 'dve': nc.vector, 'pool': nc.gpsimd, 'sp': nc.sync}
        s.nsem = 0
        s.allsems = []
        s.freelist = []
        s.taken = []
        s.clk = {e: s.sem("clk_" + e) for e in ['pe', 'act', 'dve', 'pool']}
        s.waited = {}
        s.uid = 0

    def sem(s, name=None):
        if s.freelist:
            sm = s.freelist.pop()
        else:
            s.nsem += 1
            sm = Sem(s.es.enter_context(s.nc.semaphore("s%d_%s" % (s.nsem, name or ""))))
            s.allsems.append(sm)
        s.taken.append(sm)
        return sm

    def mark(s):
        return len(s.taken)

    def release(s, mark):
        while len(s.taken) > mark:
            s.freelist.append(s.taken.pop())

    def name(s, base):
        s.uid += 1
        return "%s_%d" % (base, s.uid)

    def wait(s, eng, tok):
        if tok is None:
            return
        sem, val = tok
        if val <= 0:
            return
        key = (eng, id(sem))
        if s.waited.get(key, 0) >= val:
            return
        s.waited[key] = val
        s.engs[eng].wait_ge(sem.h, val)

    def op(s, eng, fn, deps=(), sig=True):
        for d in deps:
            s.wait(eng, d)
        ins = fn(s.engs[eng])
        if sig:
            c = s.clk[eng]
            c.n += 1
            ins.then_inc(c.h, 1)
            return (c, c.n)
        return None

    def dma(s, q, out, in_, sem, deps=()):
        for d in deps:
            s.wait(q, d)
        sem.n += 16
        s.engs[q].dma_start(out=out, in_=in_).then_inc(sem.h, 16)
        return (sem, sem.n)

    def barrier(s):
        toks = [(c, c.n) for c in s.allsems]
        for e in s.engs:
            for t in toks:
                s.wait(e, t)


class Ring:
    def __init__(s, P, es, name, shape, dtype, n, with_sems=True):
        s.n = n
        s.bufs = [es.enter_context(P.nc.sbuf_tensor(P.name(name), shape, dtype)) for _ in range(n)]
        s.sems = [P.sem(name) for _ in range(n)] if with_sems else None
        s.free = [None] * n
        s.i = 0

    def next(s):
        k = s.i % s.n
        s.i += 1
        return k, s.bufs[k], s.free[k]


def build(cfg):
    nc = bass.Bass("TRN2", target_bir_lowering=False)
    D, S, NB, KC, L = cfg.D, cfg.S, cfg.NB, cfg.KC, cfg.L
    H, PW, SBW, HGW, INW = cfg.H, cfg.PW, cfg.SBW, cfg.HGW, cfg.INW
    NT = cfg.NT

    def din(name, shape):
        return nc.dram_tensor(name, list(shape), F32, kind="ExternalInput")

    x_in = din("x", [NB, S, D])
    c_in = din("c", [NB, D])
    w_ada = din("w_ada", [L, D, 6 * D])
    b_ada = din("b_ada", [L, 6 * D])
    norm_mix = din("norm_mix", [L, D])
    norm_ffn = din("norm_ffn", [L, D])
    w_in = din("w_in", [L, D, INW])
    w_pool = din("w_pool", [L, 4, cfg.PG, cfg.PG])
    pool_scale = din("pool_scale", [L, PW])
    lb_logits = din("lb_logits", [L, HGW])
    hg_norm = din("hg_norm", [L, 128])
    w_br_pool = din("w_br_pool", [L, PW, D])
    w_br_sb = din("w_br_sb", [L, SBW, D])
    w_br_hg = din("w_br_hg", [L, HGW, D])
    w_out = din("w_out", [L, D, D])
    ffn_w1 = din("ffn_w1", [cfg.ND, D, cfg.DFF])
    ffn_w3 = din("ffn_w3", [cfg.ND, D, cfg.DFF])
    ffn_w2 = din("ffn_w2", [cfg.ND, cfg.DFF, D])
    w_router = din("w_router", [max(cfg.NM, 1), D, cfg.NE])
    b_router = din("b_router", [max(cfg.NM, 1), cfg.NE])
    moe_w1 = din("moe_w1", [max(cfg.NM, 1), cfg.NE, D, cfg.DE])
    moe_w3 = din("moe_w3", [max(cfg.NM, 1), cfg.NE, D, cfg.DE])
    moe_w2 = din("moe_w2", [max(cfg.NM, 1), cfg.NE, cfg.DE, D])
    final_norm = din("final_norm", [D])
    consts = din("consts", [128, cfg.NCONST])
    out = nc.dram_tensor("out", [NB, S, D], F32, kind="ExternalOutput")

    skind = "ExternalOutput" if cfg.debug else "Internal"

    def scratch(name, shape, dt):
        if cfg.debug:
            return nc.dram_tensor(name, list(shape), dt, kind="ExternalOutput")
        return nc.dram_tensor(name, list(shape), dt)

    xT = scratch("xT", [D, S], F32)
    proj = scratch("proj", [INW, S], F32)
    ymix = scratch("ymix", [D, S], BF16)
    merged = scratch("merged", [D, S], BF16)
    hid = scratch("hid", [cfg.HID, S], BF16)

    es = ExitStack()
    with es:
        P = Prog(nc, es)

        def sb(name, shape, dt, stack=None):
            return (stack or es).enter_context(nc.sbuf_tensor(P.name(name), list(shape), dt))

        def ps(name, shape, dt, stack):
            return stack.enter_context(nc.psum_tensor(P.name(name), list(shape), dt))

        from contextlib import contextmanager

        @contextmanager
        def phase():
            mk = P.mark()
            with ExitStack() as st_:
                yield st_
                P.barrier()
            P.release(mk)

        cst = sb("cst", [128, 512 + 2048], F32)
        identb = sb("identb", [128, 128], BF16)
        csem = P.sem("c")
        t_c = P.dma('sp', cst[:], consts[:, 0:512 + 2048], csem)
        ident = cst[:, 0:128]
        tri = cst[:, 128:256]
        ones = cst[:, 256:384]
        hgm = cst[:, 384:512]
        attm = [cst[:, 512 + d * 512: 512 + (d + 1) * 512] for d in range(4)]
        t_ib = P.op('dve', lambda e: e.tensor_copy(identb[:], ident), deps=[t_c])
        hgmi = sb("hgmi", [128, 128], mybir.dt.int32)
        t_hi = P.op('dve', lambda e: e.tensor_copy(hgmi[:], hgm), deps=[t_c])

        NJ = 6 * KC
        modv = sb("modv", [128, L, NJ, NB], F32)
        gmix = sb("gmix", [128, L, KC], F32)
        gffn = sb("gffn", [128, L, KC], F32)
        gfin = sb("gfin", [128, KC], F32)
        pscale = sb("pscale", [128, L, PW // 128], F32)
        lbt = sb("lbt", [128, L, H], F32)
        oml = sb("oml", [128, L, H], F32)
        hgn = sb("hgn", [128, L], F32)
        badat = sb("badat", [128, L, NJ], F32)
        modA = sb("modA", [128, L, 2, NB, KC], F32)
        cact = sb("cact", [128, NB, KC], F32)

        with phase() as st:
            rowb = Ring(P, st, "rowb", [128, 128], F32, 2)
            pst = ps("pst", [128, 2, 512], F32, st)
            pfree = [None, None]
            cnt = [0]

            def vec_to_fm(src2d, n, dst, func=None):
                k, rb, fr = rowb.next()
                t1 = P.dma('sp', rb[0:n, :], src2d, rowb.sems[k], deps=[fr])
                j = cnt[0] % 2
                cnt[0] += 1
                t2 = P.op('pe', lambda e: e.transpose(pst[:, j, 0:n], rb[0:n, :], ident[0:n, 0:n]),
                          deps=[t1, t_c, pfree[j]])
                if func is None:
                    t3 = P.op('dve', lambda e: e.tensor_copy(dst, pst[:, j, 0:n]), deps=[t2])
                else:
                    t3 = P.op('act', lambda e: e.activation(out=dst, in_=pst[:, j, 0:n], func=func), deps=[t2])
                pfree[j] = t3
                rowb.free[k] = t2
                return t3

            def v2(ap1d, n):
                return ap1d.rearrange("(k p) -> k p", p=128)

            toks = []
            for l in range(L):
                toks.append(vec_to_fm(v2(norm_mix[l, :], KC), KC, gmix[:, l, :]))
                toks.append(vec_to_fm(v2(norm_ffn[l, :], KC), KC, gffn[:, l, :]))
                toks.append(vec_to_fm(v2(pool_scale[l, :], PW // 128), PW // 128, pscale[:, l, :]))
                toks.append(vec_to_fm(v2(lb_logits[l, :], H), H, lbt[:, l, :]))
                toks.append(vec_to_fm(hg_norm[l:l + 1, :], 1, hgn[:, l:l + 1]))
                for j0 in range(0, NJ, 128):
                    n = min(128, NJ - j0)
                    toks.append(vec_to_fm(b_ada[l, j0 * 128:(j0 + n) * 128].rearrange("(k p) -> k p", p=128), n,
                                          badat[:, l, j0:j0 + n]))
            toks.append(vec_to_fm(v2(final_norm[:], KC), KC, gfin[:, :]))
            for b in range(NB):
                toks.append(vec_to_fm(v2(c_in[b, :], KC), KC, cact[:, b, :], func=AF.Silu))
            with phase() as st2:
                ex = sb("lbex", [128, L, H], F32, st2)
                sm = sb("lbsm", [128, H], F32, st2)
                tl = None
                for l in range(L):
                    tl = P.op('act', lambda e: e.activation(out=ex[:, l, :], in_=lbt[:, l, :], func=AF.Exp),
                              deps=toks + [tl])
                tl = P.op('dve', lambda e: e.tensor_copy(sm[:], ex[:, 0, :]), deps=[tl])
                for l in range(1, L):
                    tl = P.op('dve', lambda e: e.tensor_tensor(out=sm[:], in0=sm[:], in1=ex[:, l, :], op=ALU.add), deps=[tl])
                tl = P.op('dve', lambda e: e.reciprocal(sm[:], sm[:]), deps=[tl])
                tl = P.op('dve', lambda e: e.memset(lbt[:, 0, :], 0.0), deps=[tl])
                for l in range(1, L):
                    tl = P.op('dve', lambda e: e.tensor_tensor(out=ex[:, l, :], in0=ex[:, l, :], in1=sm[:], op=ALU.mult), deps=[tl])
                    tl = P.op('dve', lambda e: e.tensor_tensor(out=lbt[:, l, :], in0=lbt[:, l - 1, :], in1=ex[:, l, :], op=ALU.add), deps=[tl])
                tl = P.op('dve', lambda e: e.tensor_scalar(out=oml[:], in0=lbt[:], scalar1=-1.0, scalar2=1.0,
                                                           op0=ALU.mult, op1=ALU.add), deps=[tl])
                P.barrier()

            WB = 256
            wring = Ring(P, st, "wada", [128, KC, WB], F32, 2)
            pmod = ps("pmod", [128, 256, 4], F32, st)
            for l in range(L):
                tlast = None
                for c0 in range(0, 6 * D, WB):
                    k, wb_, fr = wring.next()
                    t1 = P.dma('sp', wb_[:], w_ada[l, :, c0:c0 + WB].rearrange("(k p) n -> p k n", p=128),
                               wring.sems[k], deps=[fr])
                    for m in range(WB // 128):
                        j = c0 // 128 + m
                        for kc in range(KC):
                            tlast = P.op('pe', lambda e: e.matmul(pmod[:, j, 0:NB], wb_[:, kc, m * 128:(m + 1) * 128],
                                                                  cact[:, :, kc], start=(kc == 0), stop=(kc == KC - 1)),
                                         deps=[t1] + toks, sig=(kc == KC - 1))
                    wring.free[k] = tlast
                for b in range(NB):
                    P.op('dve', lambda e: e.tensor_tensor(out=modv[:, l, :, b], in0=pmod[:, 0:NJ, b], in1=badat[:, l, :],
                                                          op=ALU.add), deps=[tlast])
                P.barrier()
            for l in range(L):
                for w in range(2):
                    g = gmix if w == 0 else gffn
                    for b in range(NB):
                        P.op('dve', lambda e: e.scalar_tensor_tensor(
                            out=modA[:, l, w, b, :], in0=modv[:, l, (3 * w + 1) * KC:(3 * w + 2) * KC, b], scalar=1.0,
                            in1=g[:, l, :], op0=ALU.add, op1=ALU.mult))
            P.barrier()

        def mod_shift(l, w, b):
            return lambda kc: modv[:, l, (3 * w) * KC + kc, b:b + 1]

        def mod_gate(l, w, b):
            return lambda kc: modv[:, l, (3 * w + 2) * KC + kc, b:b + 1]

        def norm_loader(st, scaleA, shiftB, router=None):
            xt = sb("xt", [128, KC, 512], F32, st)
            sqr = Ring(P, st, "sq", [128, 512], F32, 2, with_sems=False)
            rstd = sb("rstd", [128, 512], F32, st)
            psn = ps("psn", [128, 512], F32, st)
            xsem = P.sem("xt")
            state = {'xfree': []}

            def load(tt, hb, deps):
                tok = slice(tt * 512, (tt + 1) * 512)
                t_ld = None
                for k0 in range(0, KC, 16):
                    k1 = min(KC, k0 + 16)
                    t_ld = P.dma('sp', xt[:, k0:k1, :], xT[k0 * 128:k1 * 128, tok].rearrange("(k p) t -> p k t", p=128), xsem,
                                 deps=state['xfree'] if k0 == 0 else ())
                tm = None
                for kc in range(KC):
                    k, sq, fr = sqr.next()
                    t1 = P.op('act', lambda e: e.activation(out=sq[:], in_=xt[:, kc, :], func=AF.Square), deps=[t_ld, fr])
                    tm = P.op('pe', lambda e: e.matmul(psn[:], ones, sq[:], start=(kc == 0), stop=(kc == KC - 1)),
                              deps=[t1, t_c])
                    sqr.free[k] = tm
                t2 = P.op('dve', lambda e: e.tensor_scalar(out=rstd[:], in0=psn[:], scalar1=1.0 / D, scalar2=EPS,
                                                           op0=ALU.mult, op1=ALU.add), deps=[tm])
                t2 = P.op('act', lambda e: e.activation(out=rstd[:], in_=rstd[:], func=AF.Sqrt), deps=[t2])
                t2 = P.op('dve', lambda e: e.reciprocal(rstd[:], rstd[:]), deps=[t2])
                tl = []
                for kc in range(KC):
                    t3 = P.op('dve', lambda e: e.tensor_tensor(out=xt[:, kc, :], in0=xt[:, kc, :], in1=rstd[:], op=ALU.mult),
                              deps=[t2])
                    t4 = P.op('act', lambda e: e.activation(out=xt[:, kc, :], in_=xt[:, kc, :], func=AF.Identity,
                                                            scale=scaleA(kc), bias=shiftB(kc)), deps=[t3])
                    tl.append(t4)
                t5 = None
                NPc = 4 if KC >= 4 else 1
                stp = KC // NPc
                for i in range(NPc):
                    t5 = P.op('pool', lambda e: e.tensor_copy(hb[:, i * stp:(i + 1) * stp, :], xt[:, i * stp:(i + 1) * stp, :]),
                              deps=[tl[(i + 1) * stp - 1]] + list(deps))
                state['xfree'] = [t5]
                if router is not None:
                    tr = router(tt, xt, tl[-1])
                    state['xfree'] = [t5, tr]
                return t5
            return load

        def dram_loader(st, src, KCin, row0=0):
            sem = P.sem("hl")

            def load(tt, hb, deps):
                tok = slice(tt * 512, (tt + 1) * 512)
                t = None
                for k0 in range(0, KCin, 16):
                    k1 = min(KCin, k0 + 16)
                    t = P.dma('sp', hb[:, k0:k1, :], src[row0 + k0 * 128:row0 + k1 * 128, tok].rearrange("(k p) t -> p k t", p=128), sem,
                              deps=deps if k0 == 0 else ())
                return t
            return load

        def linear(st, groups, ncols, NBc, KCin, load_h, epilogue, ntiles=NT, KSUB=8, TW=1, NSETS=2):
            ng = len(groups)
            nm = NBc // 128
            hb = sb("hb", [128, KCin, 512 * TW], BF16, st)
            stg = Ring(P, st, "wst", [128, KSUB, NBc], F32, 3)
            wbf = Ring(P, st, "wbf", [128, KSUB, NBc], BF16, 3, with_sems=False)
            pacc = ps("pacc", [128, NSETS, ng, nm, TW, 512], F32, st)
            pfree = [[] for _ in range(NSETS)]
            hfree = None
            ncb = ncols // NBc
            assert ncols % NBc == 0 and ntiles % TW == 0
            it = 0
            for tp in range(ntiles // TW):
                t_hs = []
                for w in range(TW):
                    t_hs.append(load_h(tp * TW + w, hb[:, :, w * 512:(w + 1) * 512], [hfree]))
                tlast = None
                for cb in range(ncb):
                    pset = it % NSETS
                    it += 1
                    c0 = cb * NBc
                    for g, (W, koff) in enumerate(groups):
                        nk = W.shape[0] // 128
                        for k0 in range(0, nk, KSUB):
                            kn = min(KSUB, nk - k0)
                            ks, sbuf_, sfr = stg.next()
                            t1 = P.dma('sp', sbuf_[:, 0:kn, :],
                                       W[k0 * 128:(k0 + kn) * 128, c0:c0 + NBc].rearrange("(k p) n -> p k n", p=128),
                                       stg.sems[ks], deps=[sfr])
                            kb, wb_, bfr = wbf.next()
                            t2 = P.op('pool', lambda e: e.tensor_copy(wb_[:, 0:kn, :], sbuf_[:, 0:kn, :]), deps=[t1, bfr])
                            stg.free[ks] = t2
                            for m in range(nm):
                                for w in range(TW):
                                    for kk in range(kn):
                                        kc = k0 + kk
                                        last = (m == nm - 1 and w == TW - 1 and kk == kn - 1)
                                        tlast = P.op('pe', lambda e: e.matmul(
                                            pacc[:, pset, g, m, w, :], wb_[:, kk, m * 128:(m + 1) * 128],
                                            hb[:, koff + kc, w * 512:(w + 1) * 512],
                                            start=(kc == 0), stop=(kc == nk - 1)),
                                            deps=[t2] + t_hs + pfree[pset], sig=last)
                            wbf.free[kb] = tlast
                    pf = []
                    for w in range(TW):
                        r_ = epilogue(tp * TW + w, cb, [[pacc[:, pset, g, m, w, :] for m in range(nm)] for g in range(ng)], tlast)
                        pf.extend(r_ if isinstance(r_, list) else [r_])
                    pfree[pset] = pf
                hfree = tlast
            P.barrier()

        def phase_load_x(b):
            with phase() as st:
                xr = Ring(P, st, "xrow", [128, D], F32, 2)
                xo = Ring(P, st, "xo", [128, KC, 128], F32, 2)
                ptp = ps("ptp", [128, 2, 4, 128], F32, st)
                pfree = [None, None]
                cnt = 0
                for tb in range(S // 128):
                    k, rb, fr = xr.next()
                    t1 = P.dma('sp', rb[:], x_in[b, tb * 128:(tb + 1) * 128, :], xr.sems[k], deps=[fr])
                    ko, ob, ofr = xo.next()
                    tl = None
                    lastd = {}
                    for k4 in range(0, KC, 4):
                        j = cnt % 2
                        cnt += 1
                        tp = None
                        for i in range(4):
                            tp = P.op('pe', lambda e: e.transpose(ptp[:, j, i, :], rb[:, (k4 + i) * 128:(k4 + i + 1) * 128], ident),
                                      deps=[t1, t_c, pfree[j]], sig=(i == 3))
                        eng = 'act' if (k4 // 4) % 2 == 0 else 'dve'
                        if eng == 'act':
                            tl = P.op('act', lambda e: e.activation(out=ob[:, k4:k4 + 4, :], in_=ptp[:, j, :, :], func=AF.Copy),
                                      deps=[tp, ofr])
                        else:
                            tl = P.op('dve', lambda e: e.tensor_copy(ob[:, k4:k4 + 4, :], ptp[:, j, :, :]), deps=[tp, ofr])
                        pfree[j] = tl
                        lastd[eng] = tl
                    xr.free[k] = tp
                    t_o = P.dma('pool', xT[:, tb * 128:(tb + 1) * 128].rearrange("(k p) t -> p k t", p=128), ob[:],
                                xo.sems[ko], deps=list(lastd.values()))
                    xo.free[ko] = t_o
                P.barrier()

        def evac_store(st, dst, row0):
            stg = Ring(P, st, "ev", [128, 512], F32, 4)

            def epi(tt, cb, pss, tdep):
                tok = slice(tt * 512, (tt + 1) * 512)
                tl = None
                lastt = {}
                nm = len(pss[0])
                for m, p_ in enumerate(pss[0]):
                    k, sbuf_, fr = stg.next()
                    r0 = row0 + (cb * nm + m) * 128
                    if m % 2 == 0:
                        tl = P.op('act', lambda e: e.activation(out=sbuf_[:], in_=p_, func=AF.Copy), deps=[tdep, fr])
                        q = 'act'
                    else:
                        tl = P.op('dve', lambda e: e.tensor_copy(sbuf_[:], p_), deps=[tdep, fr])
                        q = 'pool'
                    stg.free[k] = P.dma(q, dst[r0:r0 + 128, tok], sbuf_[:], stg.sems[k], deps=[tl])
                    lastt[q] = tl
                return list(lastt.values())
            return epi

        def phase_A(b, l):
            with phase() as st:
                ld = norm_loader(st, lambda kc: modA[:, l, 0, b, kc:kc + 1], mod_shift(l, 0, b))
                linear(st, [(w_in[l, :, :], 0)], INW, 128, KC, ld, evac_store(st, proj, 0), TW=2)

        def phase_pool(b, l):
            PGC = cfg.PG // 128
            with phase() as st:
                u = sb("pu", [128, PGC, S], F32, st)
                ta = sb("pta", [128, S], F32, st)
                tb_ = sb("ptb", [128, S], F32, st)
                inv = sb("pinv", [128, S], F32, st)
                pl = sb("ppl", [128, PGC, S], BF16, st)
                wf = sb("pwf", [128, PGC, cfg.PG], F32, st)
                wbf_ = sb("pwb", [128, PGC, cfg.PG], BF16, st)
                ob = Ring(P, st, "pob", [128, 512], BF16, 2)
                pp = ps("ppp", [128, 2, 512], F32, st)
                pfree = [None, None]
                lsem = P.sem("pl")
                cnt = 0
                for g, w in enumerate(POOL_WINDOWS):
                    t0 = P.dma('sp', u[:], proj[g * cfg.PG:(g + 1) * cfg.PG, :].rearrange("(k p) t -> p k t", p=128), lsem)
                    t0 = P.dma('sp', inv[:], consts[:, cfg.c_inv + g * S: cfg.c_inv + (g + 1) * S], lsem)
                    t0 = P.dma('sp', wf[:], w_pool[l, g, :, :].rearrange("(k p) n -> p k n", p=128), lsem)
                    tw = P.op('pool', lambda e: e.tensor_copy(wbf_[:], wf[:]), deps=[t0])
                    for c_ in range(PGC):
                        src = u[:, c_, :]
                        cur, nxt = ta, tb_
                        sh = 1
                        tl = t0
                        first = True
                        while sh < w:
                            a_in = src if first else cur[:]
                            t1 = P.op('dve', lambda e: e.tensor_tensor(out=nxt[:, sh:], in0=a_in[:, sh:], in1=a_in[:, :S - sh], op=ALU.add),
                                      deps=[tl])
                            tl = P.op('dve', lambda e: e.tensor_copy(nxt[:, 0:sh], a_in[:, 0:sh]), deps=[t1])
                            cur, nxt = nxt, cur
                            sh *= 2
                            first = False
                        t2 = P.op('dve', lambda e: e.tensor_tensor(out=cur[:], in0=cur[:], in1=inv[:], op=ALU.mult), deps=[tl])
                        tl = P.op('dve', lambda e: e.tensor_tensor(out=pl[:, c_, :], in0=cur[:], in1=src, op=ALU.subtract), deps=[t2])
                    for dch in range(PGC):
                        for tt in range(NT):
                            j = cnt % 2
                            cnt += 1
                            tm = None
                            for c_ in range(PGC):
                                tm = P.op('pe', lambda e: e.matmul(pp[:, j, :], wbf_[:, c_, dch * 128:(dch + 1) * 128],
                                                                   pl[:, c_, tt * 512:(tt + 1) * 512], start=(c_ == 0), stop=(c_ == PGC - 1)),
                                          deps=[tw, tl, pfree[j]], sig=(c_ == PGC - 1))
                            k, o_, fr = ob.next()
                            ch = g * PGC + dch
                            te = P.op('act', lambda e: e.activation(out=o_[:], in_=pp[:, j, :], func=AF.Identity,
                                                                    scale=pscale[:, l, ch:ch + 1]), deps=[tm, fr])
                            pfree[j] = te
                            ob.free[k] = P.dma('act', ymix[ch * 128:(ch + 1) * 128, tt * 512:(tt + 1) * 512], o_[:], ob.sems[k], deps=[te])
                    P.barrier()

        def phase_attn(b, l):
            scale = 128 ** -0.5
            NBLK = S // 128
            with phase() as st:
                qf = sb("aqf", [128, S], F32, st)
                kf = sb("akf", [128, S], F32, st)
                vf = sb("avf", [128, S], F32, st)
                qb = sb("aqb", [128, S], BF16, st)
                kb = sb("akb", [128, S], BF16, st)
                vb = sb("avb", [128, S], BF16, st)
                vT = sb("avT", [128, NBLK, 128], BF16, st)
                R = sb("aR", [128, 512], F32, st)
                e_r = Ring(P, st, "ae", [128, 512], F32, 2, with_sems=False)
                sp_r = Ring(P, st, "asp", [128, 512], F32, 2, with_sems=False)
                t1_r = Ring(P, st, "at1", [128, 512], F32, 2, with_sems=False)
                a_r = Ring(P, st, "aa", [128, 512], BF16, 2, with_sems=False)
                ob = Ring(P, st, "aob", [128, 512], BF16, 2)
                pz = ps("apz", [128, 2, 512], F32, st)
                ptr = ps("aptr", [128, 2, 512], F32, st)
                pon = ps("apon", [128, 2, 512], F32, st)
                po = ps("apo", [128, 512], F32, st)
                ptv = ps("aptv", [128, 4, 256], BF16, st)
                lsem = P.sem("al")
                pzf, ptrf, ponf = [None, None], [None, None], [None, None]
                pof = None
                cnt = 0
                hfree = None
                tRlast = None
                for h in range(H):
                    r0 = h * 128
                    P.dma('sp', qf[:], proj[cfg.o_q + r0: cfg.o_q + r0 + 128, :], lsem, deps=[hfree])
                    P.dma('sp', kf[:], proj[cfg.o_k + r0: cfg.o_k + r0 + 128, :], lsem)
                    t0 = P.dma('sp', vf[:], proj[cfg.o_v + r0: cfg.o_v + r0 + 128, :], lsem)
                    tq = P.op('pool', lambda e: e.tensor_copy(qb[:], qf[:]), deps=[t0])
                    tk = P.op('pool', lambda e: e.tensor_copy(kb[:], kf[:]), deps=[tq])
                    tv = P.op('dve', lambda e: e.tensor_copy(vb[:], vf[:]), deps=[t0])
                    tvT = None
                    for b4 in range(0, NBLK, 4):
                        tp = None
                        for i in range(4):
                            tp = P.op('pe', lambda e: e.transpose(ptv[:, i, 0:128], vb[:, (b4 + i) * 128:(b4 + i + 1) * 128], identb[:]),
                                      deps=[tv, t_ib, tvT], sig=(i == 3))
                        tvT = P.op('act', lambda e: e.activation(out=vT[:, b4:b4 + 4, :], in_=ptv[:, :, 0:128], func=AF.Copy), deps=[tp])
                    for TB in range(S // 512):
                        qs = slice(TB * 512, (TB + 1) * 512)
                        nkb = 4 * TB + 4
                        tR = P.op('pool', lambda e: e.memset(R[:], 0.0), deps=[hfree, tRlast])
                        tav = None
                        for idx, sbk in enumerate(range(nkb - 1, -1, -1)):
                            j = cnt % 2
                            cnt += 1
                            diag = sbk - 4 * TB
                            tz = P.op('pe', lambda e: e.matmul(pz[:, j, :], kb[:, sbk * 128:(sbk + 1) * 128], qb[:, qs], start=True, stop=True),
                                      deps=[tq, tk, pzf[j]])
                            _, e_, _ = e_r.next()
                            _, sp_, _ = sp_r.next()
                            _, t1_, _ = t1_r.next()
                            _, a_, _ = a_r.next()
                            te = P.op('act', lambda e: e.activation(out=e_[:], in_=pz[:, j, :], func=AF.Exp, scale=scale), deps=[tz, tav])
                            ts = P.op('act', lambda e: e.activation(out=sp_[:], in_=e_[:], func=AF.Ln, bias=1.0, scale=1.0), deps=[te])
                            if diag >= 0:
                                ts = P.op('dve', lambda e: e.tensor_tensor(out=sp_[:], in0=sp_[:], in1=attm[diag], op=ALU.mult), deps=[ts, t_c])
                            ttr = P.op('pe', lambda e: e.matmul(ptr[:, j, :], tri, sp_[:], start=True, stop=True), deps=[ts, ptrf[j]])
                            ton = P.op('pe', lambda e: e.matmul(pon[:, j, :], ones, sp_[:], start=True, stop=True), deps=[ts, ponf[j]])
                            t1 = P.op('dve', lambda e: e.scalar_tensor_tensor(out=t1_[:], in0=pz[:, j, :], scalar=scale, in1=sp_[:],
                                                                              op0=ALU.mult, op1=ALU.subtract), deps=[ts])
                            pzf[j] = t1
                            t2 = P.op('dve', lambda e: e.tensor_tensor(out=t1_[:], in0=t1_[:], in1=ptr[:, j, :], op=ALU.subtract), deps=[t1, ttr])
                            ptrf[j] = t2
                            t3 = P.op('dve', lambda e: e.tensor_tensor(out=t1_[:], in0=t1_[:], in1=R[:], op=ALU.subtract), deps=[t2, tR])
                            tR = P.op('dve', lambda e: e.tensor_tensor(out=R[:], in0=R[:], in1=pon[:, j, :], op=ALU.add), deps=[t3, ton])
                            ponf[j] = tR
                            ta_ = P.op('act', lambda e: e.activation(out=a_[:], in_=t1_[:], func=AF.Exp), deps=[t3])
                            if diag >= 0:
                                ta_ = P.op('pool', lambda e: e.tensor_tensor(out=a_[:], in0=a_[:], in1=attm[diag], op=ALU.mult), deps=[ta_, t_c])
                            tav = P.op('pe', lambda e: e.matmul(po[:], vT[:, sbk, :], a_[:], start=(idx == 0), stop=(idx == nkb - 1)),
                                       deps=[ta_, tvT, pof])
                        k, o_, fr = ob.next()
                        te2 = P.op('act', lambda e: e.activation(out=o_[:], in_=po[:], func=AF.Copy), deps=[tav, fr])
                        pof = te2
                        ch = PW // 128 + h
                        ob.free[k] = P.dma('act', ymix[ch * 128:(ch + 1) * 128, qs], o_[:], ob.sems[k], deps=[te2])
                        hfree = tav
                        tRlast = tR
                P.barrier()

        def phase_hgrn(b, l):
            NBLK = S // 128
            NCH = S // 64
            with phase() as st:
                qf = sb("hqf", [128, S], F32, st)
                ff = sb("hff", [128, S], F32, st)
                i_f = sb("hif", [128, S], F32, st)
                gf = sb("hgf", [128, S], F32, st)
                scm = sb("hscm", [128, S], F32, st)
                bb = sb("hbb", [128, S], F32, st)
                eb = sb("heb", [128, S], F32, st)
                tmp = sb("htmp", [128, S], F32, st)
                Qt = sb("hQt", [128, S], BF16, st)
                Kt = sb("hKt", [128, S], BF16, st)
                ib = sb("hib", [128, S], BF16, st)
                KtT = sb("hKtT", [128, NBLK, 128], BF16, st)
                iT = sb("hiT", [128, NBLK, 128], BF16, st)
                o_f = sb("hof", [128, S], F32, st)
                Sf = sb("hSf", [128, 128], F32, st)
                bmid = sb("hbmid", [128, NCH, 1], F32, st)
                ebm = sb("hebm", [128, NCH, 1], F32, st)
                Sin = Ring(P, st, "hSin", [128, 128], F32, 2, with_sems=False)
                Sb = Ring(P, st, "hSb", [128, 128], BF16, 3, with_sems=False)
                scb = Ring(P, st, "hscb", [128, 128], BF16, 2, with_sems=False)
                ob = Ring(P, st, "hob", [128, 512], BF16, 2)
                rs = sb("hrs", [128, 512], F32, st)
                psc = ps("hpsc", [128, 2, 512], F32, st)
                pou = ps("hpou", [128, 2, 512], F32, st)
                pst_ = ps("hpst", [128, 2, 512], F32, st)
                pnr = ps("hpnr", [128, 512], F32, st)
                pt = ps("hpt", [128, 4, 256], BF16, st)
                lsem = P.sem("hl")
                t_m = P.dma('sp', scm[:], consts[:, cfg.c_scan:cfg.c_scan + S], lsem)
                tz0 = None
                for bf_ in scb.bufs:
                    tz0 = P.op('dve', lambda e: e.memset(bf_[:], 0.0), deps=[tz0])
                hfree = None
                pscf, pouf, pstf = [None, None], [None, None], [None, None]
                pnrf = None
                cnt = 0
                for h in range(H):
                    r0 = h * 128
                    P.dma('sp', qf[:], proj[cfg.o_qh + r0: cfg.o_qh + r0 + 128, :], lsem, deps=[hfree])
                    P.dma('sp', ff[:], proj[cfg.o_fh + r0: cfg.o_fh + r0 + 128, :], lsem)
                    P.dma('sp', i_f[:], proj[cfg.o_ih + r0: cfg.o_ih + r0 + 128, :], lsem)
                    t0 = P.dma('sp', gf[:], proj[cfg.o_gh + r0: cfg.o_gh + r0 + 128, :], lsem)
                    t1 = P.op('act', lambda e: e.activation(out=ff[:], in_=ff[:], func=AF.Sigmoid), deps=[t0])
                    t1 = P.op('dve', lambda e: e.tensor_scalar(out=ff[:], in0=ff[:], scalar1=oml[:, l, h:h + 1], scalar2=lbt[:, l, h:h + 1],
                                                               op0=ALU.mult, op1=ALU.add), deps=[t1])
                    t2 = P.op('act', lambda e: e.activation(out=tmp[:], in_=ff[:], func=AF.Ln), deps=[t1])
                    t3 = P.op('dve', lambda e: e.tensor_tensor_scan(out=bb[:], data0=scm[:], data1=tmp[:], initial=0.0,
                                                                    op0=ALU.mult, op1=ALU.add), deps=[t2, t_m])
                    bb3 = bb[:].rearrange("p (c t) -> p c t", t=64)
                    t3 = P.op('dve', lambda e: e.tensor_copy(bmid[:], bb3[:, :, 31:32]), deps=[t3])
                    t3b = P.op('act', lambda e: e.activation(out=ebm[:], in_=bmid[:], func=AF.Exp), deps=[t3])
                    t3 = P.op('dve', lambda e: e.tensor_tensor(out=bb3, in0=bb3, in1=bmid[:].to_broadcast([128, NCH, 64]), op=ALU.subtract), deps=[t3])
                    t4 = P.op('act', lambda e: e.activation(out=eb[:], in_=bb[:], func=AF.Exp), deps=[t3, t3b])
                    t5 = P.op('act', lambda e: e.activation(out=tmp[:], in_=bb[:], func=AF.Exp, scale=-1.0), deps=[t3])
                    t6 = P.op('dve', lambda e: e.tensor_scalar(out=ff[:], in0=ff[:], scalar1=-1.0, scalar2=1.0, op0=ALU.mult, op1=ALU.add),
                              deps=[t2])
                    tK = P.op('dve', lambda e: e.tensor_tensor(out=Kt[:], in0=ff[:], in1=tmp[:], op=ALU.mult), deps=[t5, t6])
                    t7 = P.op('act', lambda e: e.activation(out=qf[:], in_=qf[:], func=AF.Silu), deps=[t0])
                    tQ = P.op('dve', lambda e: e.tensor_tensor(out=Qt[:], in0=qf[:], in1=eb[:], op=ALU.mult), deps=[t7, t4])
                    ti = P.op('pool', lambda e: e.tensor_copy(ib[:], i_f[:]), deps=[t0])
                    tT = None
                    for (src, dst, tsrc) in ((Kt, KtT, tK), (ib, iT, ti)):
                        for b4 in range(0, NBLK, 4):
                            tp = None
                            for i in range(4):
                                tp = P.op('pe', lambda e: e.transpose(pt[:, i, 0:128], src[:, (b4 + i) * 128:(b4 + i + 1) * 128], identb[:]),
                                          deps=[tsrc, t_ib, tT], sig=(i == 3))
                            tT = P.op('act', lambda e: e.activation(out=dst[:, b4:b4 + 4, :], in_=pt[:, :, 0:128], func=AF.Copy), deps=[tp])
                    tS = P.op('dve', lambda e: e.memset(Sf[:], 0.0), deps=[hfree])
                    to_all = None
                    tSb = None
                    for blk in range(NBLK):
                        j = cnt % 2
                        cnt += 1
                        bs = slice(blk * 128, (blk + 1) * 128)
                        tsc = P.op('pe', lambda e: e.matmul(psc[:, j, 0:128], Kt[:, bs], Qt[:, bs], start=True, stop=True), deps=[tK, tQ, pscf[j]])
                        _, sc_, _ = scb.next()
                        tm_ = P.op('dve', lambda e: e.copy_predicated(out=sc_[:], mask=hgmi[:], data=psc[:, j, 0:128]), deps=[tsc, t_hi, tz0])
                        pscf[j] = tm_
                        tin = P.op('pe', lambda e: e.matmul(pou[:, j, 0:128], iT[:, blk, :], sc_[:], start=True, stop=False), deps=[tm_, tT, pouf[j]])
                        for hf in range(2):
                            c_ = 2 * blk + hf
                            cs = slice(c_ * 64, (c_ + 1) * 64)
                            pr = slice(hf * 64, (hf + 1) * 64)
                            _, sin_, _ = Sin.next()
                            tSi = P.op('dve', lambda e: e.tensor_scalar(out=sin_[:], in0=Sf[:], scalar1=ebm[:, c_, :], scalar2=None, op0=ALU.mult),
                                       deps=[tS, t3b, tSb])
                            _, sbcur, _ = Sb.next()
                            tSb = P.op('pool', lambda e: e.tensor_copy(sbcur[:], sin_[:]), deps=[tSi, tin])
                            tin = P.op('pe', lambda e: e.matmul(pou[:, j, hf * 64:(hf + 1) * 64], sbcur[:], Qt[:, cs], start=False, stop=(hf == 1)),
                                       deps=[tSb, tin])
                            tpm = P.op('pe', lambda e: e.matmul(pst_[:, hf, 0:128], KtT[pr, blk, :], iT[pr, blk, :], start=True, stop=True),
                                       deps=[tT, pstf[hf]])
                            tS = P.op('dve', lambda e: e.tensor_tensor(out=Sf[:], in0=sin_[:], in1=pst_[:, hf, 0:128], op=ALU.add), deps=[tpm, tSi])
                            pstf[hf] = tS
                            tS = P.op('dve', lambda e: e.tensor_scalar(out=Sf[:], in0=Sf[:], scalar1=eb[:, c_ * 64 + 63:c_ * 64 + 64], scalar2=None,
                                                                       op0=ALU.mult), deps=[tS, t4])
                        to_all = P.op('act', lambda e: e.activation(out=o_f[:, bs], in_=pou[:, j, 0:128], func=AF.Copy), deps=[tin])
                        pouf[j] = to_all
                    tg = P.op('act', lambda e: e.activation(out=gf[:], in_=gf[:], func=AF.Silu), deps=[t0])
                    tg = P.op('dve', lambda e: e.tensor_scalar(out=gf[:], in0=gf[:], scalar1=hgn[:, l:l + 1], scalar2=None, op0=ALU.mult), deps=[tg])
                    for tt in range(NT):
                        ts_ = slice(tt * 512, (tt + 1) * 512)
                        tq2 = P.op('act', lambda e: e.activation(out=tmp[:, ts_], in_=o_f[:, ts_], func=AF.Square), deps=[to_all, tK])
                        tn = P.op('pe', lambda e: e.matmul(pnr[:], ones, tmp[:, ts_], start=True, stop=True), deps=[tq2, pnrf])
                        tr = P.op('dve', lambda e: e.tensor_scalar(out=rs[:], in0=pnr[:], scalar1=1.0 / 128, scalar2=EPS, op0=ALU.mult, op1=ALU.add), deps=[tn])
                        pnrf = tr
                        tr = P.op('act', lambda e: e.activation(out=rs[:], in_=rs[:], func=AF.Sqrt), deps=[tr])
                        tr = P.op('dve', lambda e: e.reciprocal(rs[:], rs[:]), deps=[tr])
                        tr = P.op('dve', lambda e: e.tensor_tensor(out=rs[:], in0=rs[:], in1=o_f[:, ts_], op=ALU.mult), deps=[tr])
                        k, o_, fr = ob.next()
                        tr = P.op('dve', lambda e: e.tensor_tensor(out=o_[:], in0=rs[:], in1=gf[:, ts_], op=ALU.mult), deps=[tr, tg, fr])
                        ch = (PW + SBW) // 128 + h
                        ob.free[k] = P.dma('sp', ymix[ch * 128:(ch + 1) * 128, ts_], o_[:], ob.sems[k], deps=[tr])
                        hfree = tr
                P.barrier()

        def phase_C(b, l):
            with phase() as st:
                gl = Ring(P, st, "cgl", [128, 3, 512], F32, 2)
                acc = Ring(P, st, "cacc", [128, 512], F32, 2, with_sems=False)
                tm_ = Ring(P, st, "ctm", [128, 512], F32, 2, with_sems=False)
                ob = Ring(P, st, "cob", [128, 512], BF16, 2)

                def epi(tt, cb, pss, tdep):
                    tok = slice(tt * 512, (tt + 1) * 512)
                    m = cb
                    k, g_, fr = gl.next()
                    src = proj[cfg.o_gate:cfg.o_gate + 3 * D, tok].rearrange("(r d) t -> d r t", r=3)[m * 128:(m + 1) * 128]
                    t0 = P.dma('pool', g_[:], src, gl.sems[k], deps=[fr])
                    ts = P.op('act', lambda e: e.activation(out=g_[:], in_=g_[:], func=AF.Sigmoid), deps=[t0])
                    _, a_, _ = acc.next()
                    _, t_, _ = tm_.next()
                    ko, o_, ofr = ob.next()
                    t1 = P.op('dve', lambda e: e.tensor_tensor(out=a_[:], in0=g_[:, 0, :], in1=pss[0][0], op=ALU.mult), deps=[ts, tdep])
                    t2 = P.op('dve', lambda e: e.tensor_tensor(out=t_[:], in0=g_[:, 1, :], in1=pss[1][0], op=ALU.mult), deps=[t1])
                    t2 = P.op('dve', lambda e: e.tensor_tensor(out=a_[:], in0=a_[:], in1=t_[:], op=ALU.add), deps=[t2])
                    t3 = P.op('dve', lambda e: e.tensor_tensor(out=t_[:], in0=g_[:, 2, :], in1=pss[2][0], op=ALU.mult), deps=[t2])
                    t3 = P.op('dve', lambda e: e.tensor_tensor(out=o_[:], in0=a_[:], in1=t_[:], op=ALU.add), deps=[t3, ofr])
                    gl.free[k] = t3
                    ob.free[ko] = P.dma('act', merged[m * 128:(m + 1) * 128, tok], o_[:], ob.sems[ko], deps=[t3])
                    return t3
                groups = [(w_br_pool[l, :, :], 0), (w_br_sb[l, :, :], PW // 128), (w_br_hg[l, :, :], (PW + SBW) // 128)]
                linear(st, groups, D, 128, KC, dram_loader(st, ymix, KC), epi, TW=2, NSETS=1)

        def resid_epi(st, gate):
            xr = Ring(P, st, "rx", [128, 512], F32, 3)

            def epi(tt, cb, pss, tdep):
                tok = slice(tt * 512, (tt + 1) * 512)
                tl = None
                nm = len(pss[0])
                for m, p_ in enumerate(pss[0]):
                    ch = cb * nm + m
                    k, x_, fr = xr.next()
                    t0 = P.dma('pool', x_[:], xT[ch * 128:(ch + 1) * 128, tok], xr.sems[k], deps=[fr])
                    tl = P.op('dve', lambda e: e.scalar_tensor_tensor(out=x_[:], in0=p_, scalar=gate(ch), in1=x_[:],
                                                                      op0=ALU.mult, op1=ALU.add), deps=[t0, tdep])
                    xr.free[k] = P.dma('act', xT[ch * 128:(ch + 1) * 128, tok], x_[:], xr.sems[k], deps=[tl])
                return tl
            return epi

        def phase_D(b, l):
            with phase() as st:
                linear(st, [(w_out[l, :, :], 0)], D, 256, KC, dram_loader(st, merged, KC), resid_epi(st, mod_gate(l, 0, b)), TW=2)

        def ffn_up_epi(st):
            sa = Ring(P, st, "fsa", [128, 512], F32, 2, with_sems=False)
            ob = Ring(P, st, "fob", [128, 512], BF16, 3)

            def epi(tt, cb, pss, tdep, row0=0, bc=None):
                tok = slice(tt * 512, (tt + 1) * 512)
                tl = None
                nm = len(pss[0])
                for m in range(nm):
                    _, s_, _ = sa.next()
                    k, o_, fr = ob.next()
                    t1 = P.op('act', lambda e: e.activation(out=s_[:], in_=pss[0][m], func=AF.Silu), deps=[tdep, tl])
                    if bc is None:
                        tl = P.op('dve', lambda e: e.tensor_tensor(out=o_[:], in0=s_[:], in1=pss[1][m], op=ALU.mult), deps=[t1, fr])
                    else:
                        t2 = P.op('dve', lambda e: e.tensor_tensor(out=s_[:], in0=s_[:], in1=pss[1][m], op=ALU.mult), deps=[t1])
                        tl = P.op('dve', lambda e: e.tensor_tensor(out=o_[:], in0=s_[:], in1=bc, op=ALU.mult), deps=[t2, fr])
                    r0 = row0 + (cb * nm + m) * 128
                    ob.free[k] = P.dma('act', hid[r0:r0 + 128, tok], o_[:], ob.sems[k], deps=[tl])
                return tl
            return epi

        def phase_ffn_dense(b, l):
            j = l // 2
            with phase() as st:
                ld = norm_loader(st, lambda kc: modA[:, l, 1, b, kc:kc + 1], mod_shift(l, 1, b))
                linear(st, [(ffn_w1[j, :, :], 0), (ffn_w3[j, :, :], 0)], cfg.DFF, 128, KC, ld, ffn_up_epi(st), TW=2, NSETS=1)
            KF = cfg.DFF // 128
            KH = KF // 2
            for (ka, kb_) in ((0, KH), (KH, KF)):
                with phase() as st:
                    linear(st, [(ffn_w2[j, ka * 128:kb_ * 128, :], 0)], D, 256, kb_ - ka, dram_loader(st, hid, kb_ - ka, row0=ka * 128),
                           resid_epi(st, mod_gate(l, 1, b)), TW=2)

        def phase_ffn_moe(b, l):
            j = l // 2
            NE, DE = cfg.NE, cfg.DE
            with phase() as st:
                hb = sb("mhb", [128, KC, 512], BF16, st)
                wr = sb("mwr", [128, KC, NE], F32, st)
                brt = sb("mbr", [NE, 1], F32, st)
                lg = sb("mlg", [NE, 512], F32, st)
                lgT = sb("mlgT", [128, 4, NE], F32, st)
                wk = sb("mwk", [128, 4, NE], F32, st)
                mx = sb("mmx", [128, 4, 1], F32, st)
                mx2 = sb("mmx2", [128, 4, 1], F32, st)
                cmb = sb("mcmb", [128, 4, NE], F32, st)
                cmT = sb("mcmT", [NE, 512], F32, st)
                bcs = sb("mbcs", [128, NE, 512], F32, st)
                sel = sb("msel", [NE, NE, 128], F32, st)
                plg = ps("mplg", [128, 512], F32, st)
                plt = ps("mplt", [128, 4, 128], F32, st)
                msem = P.sem("ml")
                P.dma('sp', wr[:], w_router[j, :, :].rearrange("(k p) n -> p k n", p=128), msem)
                t0 = P.dma('sp', brt[:], b_router[j, :].rearrange("(n o) -> n o", o=1), msem)
                tsel = t0
                tsel = P.op('dve', lambda e: e.memset(sel[:], 0.0), deps=[t0])
                for e_ in range(NE):
                    tsel = P.op('dve', lambda e: e.tensor_scalar(out=sel[:, e_, :], in0=sel[:, e_, :], scalar1=ident[0:NE, e_:e_ + 1],
                                                                 scalar2=None, op0=ALU.add), deps=[tsel, t_c])
                state = {'tok': None}

                def router(tt, xt, tdep):
                    tm = None
                    for kc in range(KC):
                        tm = P.op('pe', lambda e: e.matmul(plg[0:NE, :], wr[:, kc, :], xt[:, kc, :], start=(kc == 0), stop=(kc == KC - 1)),
                                  deps=[tdep, t0, state['tok']], sig=(kc == KC - 1))
                    t1 = P.op('act', lambda e: e.activation(out=lg[:], in_=plg[0:NE, :], func=AF.Identity, bias=brt[:, 0:1], scale=1.0), deps=[tm])
                    tp = None
                    for i in range(4):
                        tp = P.op('pe', lambda e: e.transpose(plt[:, i, 0:NE], lg[:, i * 128:(i + 1) * 128], ident[0:NE, 0:NE]), deps=[t1, t_c], sig=(i == 3))
                    t2 = P.op('dve', lambda e: e.tensor_copy(lgT[:], plt[:, :, 0:NE]), deps=[tp])
                    t3 = t2
                    for i in range(4):
                        L_ = lgT[:, i, :]
                        W_ = wk[:, i, :]
                        C_ = cmb[:, i, :]
                        m1 = mx[:, i, :]
                        m2 = mx2[:, i, :]
                        t3 = P.op('dve', lambda e: e.reduce_max(out=m1, in_=L_, axis=mybir.AxisListType.X), deps=[t3])
                        t3 = P.op('dve', lambda e: e.tensor_scalar(out=W_, in0=L_, scalar1=m1, scalar2=None, op0=ALU.is_ge), deps=[t3])
                        t3 = P.op('dve', lambda e: e.scalar_tensor_tensor(out=W_, in0=W_, scalar=-1e30, in1=L_, op0=ALU.mult, op1=ALU.add), deps=[t3])
                        t3 = P.op('dve', lambda e: e.reduce_max(out=m2, in_=W_, axis=mybir.AxisListType.X), deps=[t3])
                        t3 = P.op('dve', lambda e: e.tensor_scalar(out=W_, in0=L_, scalar1=m2, scalar2=None, op0=ALU.is_ge), deps=[t3])
                        t3 = P.op('dve', lambda e: e.tensor_scalar(out=C_, in0=L_, scalar1=m1, scalar2=None, op0=ALU.subtract), deps=[t3])
                        t3 = P.op('act', lambda e: e.activation(out=C_, in_=C_, func=AF.Exp), deps=[t3])
                        t3 = P.op('dve', lambda e: e.tensor_tensor(out=C_, in0=C_, in1=W_, op=ALU.mult), deps=[t3])
                        t3 = P.op('dve', lambda e: e.reduce_sum(out=m2, in_=C_, axis=mybir.AxisListType.X), deps=[t3])
                        t3 = P.op('dve', lambda e: e.reciprocal(m2, m2), deps=[t3])
                        t3 = P.op('dve', lambda e: e.tensor_scalar(out=C_, in0=C_, scalar1=m2, scalar2=None, op0=ALU.mult), deps=[t3])
                    tp = None
                    for i in range(4):
                        tp = P.op('pe', lambda e: e.transpose(plg[0:NE, i * 128:(i + 1) * 128], cmb[:, i, :], ident), deps=[t3, t1], sig=(i == 3))
                    t4 = P.op('act', lambda e: e.activation(out=cmT[:], in_=plg[0:NE, :], func=AF.Copy), deps=[tp])
                    tb_ = t4
                    for e_ in range(NE):
                        tq = P.op('pe', lambda e: e.matmul(plg[:], sel[:, e_, :], cmT[:], start=True, stop=True), deps=[t4, tsel, tb_])
                        tb_ = P.op('dve', lambda e: e.tensor_copy(bcs[:, e_, :], plg[:]), deps=[tq])
                    state['tok'] = tb_
                    return tb_

                ld = norm_loader(st, lambda kc: modA[:, l, 1, b, kc:kc + 1], mod_shift(l, 1, b), router=router)
                moe_linear_up(st, b, l, j, ld, bcs, state)
            SEG = 2
            for s0 in range(0, NE, SEG):
                with phase() as st:
                    KCs = SEG * DE // 128
                    groups = [(moe_w2[j, :, :, :].rearrange("e k n -> (e k) n")[s0 * DE:(s0 + SEG) * DE, :], 0)]

                    def ldr(st_):
                        sem = P.sem("hl2")

                        def load(tt, hb, deps):
                            tok = slice(tt * 512, (tt + 1) * 512)
                            t = None
                            for k0 in range(0, KCs, 16):
                                k1 = min(KCs, k0 + 16)
                                t = P.dma('sp', hb[:, k0:k1, :], hid[s0 * DE + k0 * 128:s0 * DE + k1 * 128, tok].rearrange("(k p) t -> p k t", p=128),
                                          sem, deps=deps if k0 == 0 else ())
                            return t
                        return load
                    linear(st, groups, D, 256, KCs, ldr(st), resid_epi(st, mod_gate(l, 1, b)), TW=2)

        def moe_linear_up(st, b, l, j, ld, bcs, state):
            NE, DE = cfg.NE, cfg.DE
            KSUB, NBc = 8, 128
            nm = 1
            hb = sb("hb", [128, KC, 512], BF16, st)
            stg = Ring(P, st, "wst", [128, KSUB, NBc], F32, 3)
            wbf = Ring(P, st, "wbf", [128, KSUB, NBc], BF16, 3, with_sems=False)
            pacc = ps("pacc", [128, 2, 2, nm, 512], F32, st)
            pfree = [None, None]
            hfree = None
            cnt = 0
            epi_up = ffn_up_epi(st)
            for tt in range(NT):
                t_h = ld(tt, hb, [hfree])
                t_r = state['tok']
                tlast = None
                for e_ in range(NE):
                    for cb in range(DE // NBc):
                        pset = cnt % 2
                        cnt += 1
                        c0 = cb * NBc
                        for g, W in enumerate((moe_w1[j, e_, :, :], moe_w3[j, e_, :, :])):
                            for k0 in range(0, KC, KSUB):
                                kn = min(KSUB, KC - k0)
                                ks, sbuf_, sfr = stg.next()
                                t1 = P.dma('sp', sbuf_[:, 0:kn, :], W[k0 * 128:(k0 + kn) * 128, c0:c0 + NBc].rearrange("(k p) n -> p k n", p=128),
                                           stg.sems[ks], deps=[sfr])
                                kb, wb_, bfr = wbf.next()
                                t2 = P.op('pool', lambda e: e.tensor_copy(wb_[:, 0:kn, :], sbuf_[:, 0:kn, :]), deps=[t1, bfr])
                                stg.free[ks] = t2
                                for m in range(nm):
                                    for kk in range(kn):
                                        kc = k0 + kk
                                        last = (m == nm - 1 and kk == kn - 1)
                                        tlast = P.op('pe', lambda e: e.matmul(pacc[:, pset, g, m, :], wb_[:, kk, m * 128:(m + 1) * 128], hb[:, kc, :],
                                                                              start=(kc == 0), stop=(kc == KC - 1)),
                                                     deps=[t2, t_h, pfree[pset]], sig=last)
                                wbf.free[kb] = tlast
                        P.wait('dve', t_r)
                        pfree[pset] = epi_up(tt, cb, [[pacc[:, pset, g, m, :] for m in range(nm)] for g in range(2)], tlast,
                                             row0=e_ * DE, bc=bcs[:, e_, :])
                hfree = tlast
                state['tok'] = tlast
            P.barrier()

        def phase_final(b):
            with phase() as st:
                hbuf = None
                xt = sb("fxt", [128, KC, 512], F32, st)
                sqr = Ring(P, st, "fsq", [128, 512], F32, 2, with_sems=False)
                rstd = sb("frs", [128, 512], F32, st)
                psn = ps("fpsn", [128, 512], F32, st)
                ptp = ps("fptp", [128, 2, 512], F32, st)
                orow = Ring(P, st, "forow", [128, D], F32, 2)
                xsem = P.sem("fx")
                xfree = None
                pfree = [None, None]
                cnt = 0
                for tt in range(NT):
                    tok = slice(tt * 512, (tt + 1) * 512)
                    t_ld = None
                    for k0 in range(0, KC, 16):
                        k1 = min(KC, k0 + 16)
                        t_ld = P.dma('sp', xt[:, k0:k1, :], xT[k0 * 128:k1 * 128, tok].rearrange("(k p) t -> p k t", p=128), xsem,
                                     deps=[xfree] if k0 == 0 else ())
                    tm = None
                    for kc in range(KC):
                        k, sq, fr = sqr.next()
                        t1 = P.op('act', lambda e: e.activation(out=sq[:], in_=xt[:, kc, :], func=AF.Square), deps=[t_ld, fr])
                        tm = P.op('pe', lambda e: e.matmul(psn[:], ones, sq[:], start=(kc == 0), stop=(kc == KC - 1)), deps=[t1, t_c])
                        sqr.free[k] = tm
                    t2 = P.op('dve', lambda e: e.tensor_scalar(out=rstd[:], in0=psn[:], scalar1=1.0 / D, scalar2=EPS, op0=ALU.mult, op1=ALU.add), deps=[tm])
                    t2 = P.op('act', lambda e: e.activation(out=rstd[:], in_=rstd[:], func=AF.Sqrt), deps=[t2])
                    t2 = P.op('dve', lambda e: e.reciprocal(rstd[:], rstd[:]), deps=[t2])
                    tl = []
                    for kc in range(KC):
                        t3 = P.op('dve', lambda e: e.scalar_tensor_tensor(out=xt[:, kc, :], in0=xt[:, kc, :], scalar=gfin[:, kc:kc + 1], in1=rstd[:],
                                                                          op0=ALU.mult, op1=ALU.mult), deps=[t2])
                        tl.append(t3)
                    for tb in range(4):
                        ko, ob_, ofr = orow.next()
                        te = None
                        for k4 in range(0, KC, 4):
                            j = cnt % 2
                            cnt += 1
                            tp = None
                            for i in range(4):
                                tp = P.op('pe', lambda e: e.transpose(ptp[:, j, i * 128:(i + 1) * 128], xt[:, k4 + i, tb * 128:(tb + 1) * 128], ident),
                                          deps=[tl[k4 + i], pfree[j]], sig=(i == 3))
                            if (k4 // 4) % 2 == 0:
                                te = P.op('act', lambda e: e.activation(out=ob_[:, k4 * 128:(k4 + 4) * 128], in_=ptp[:, j, :], func=AF.Copy), deps=[tp, ofr])
                                te_a = te
                            else:
                                te = P.op('dve', lambda e: e.tensor_copy(ob_[:, k4 * 128:(k4 + 4) * 128], ptp[:, j, :]), deps=[tp, ofr])
                                te_d = te
                            pfree[j] = te
                        deps_ = [te_a] + ([te_d] if KC > 4 else [])
                        orow.free[ko] = P.dma('pool', out[b, tt * 512 + tb * 128: tt * 512 + (tb + 1) * 128, :], ob_[:], orow.sems[ko], deps=deps_)
                        xfree = tp
                P.barrier()

        stop_after = getattr(cfg, 'stop_after', None)
        for b in range(NB):
            phase_load_x(b)
            for l in range(L):
                phase_A(b, l)
                if stop_after == 'A':
                    break
                phase_pool(b, l)
                phase_attn(b, l)
                phase_hgrn(b, l)
                if stop_after == 'B':
                    break
                phase_C(b, l)
                phase_D(b, l)
                if stop_after == 'D':
                    break
                if l % 2 == 0:
                    phase_ffn_dense(b, l)
                else:
                    phase_ffn_moe(b, l)
                if stop_after == 'F0':
                    break
            phase_final(b)
        P.barrier()
    return nc


FULL = Cfg(NB=1)
NCORES = 8 // FULL.NB


def kernel(**inputs):
    cfg = FULL
    nc = build(cfg)
    cst = make_consts(cfg)
    in_maps = []
    for ci in range(NCORES):
        m = {}
        for k, v in inputs.items():
            v = np.asarray(v)
            if k == 'x':
                m[k] = np.ascontiguousarray(v[ci * cfg.NB:(ci + 1) * cfg.NB])
            elif k == 'c':
                m[k] = np.ascontiguousarray(v[ci * cfg.NB:(ci + 1) * cfg.NB])
            else:
                m[k] = v
        m['consts'] = cst
        in_maps.append(m)
    res = run_bass_kernel_spmd(nc, in_maps, core_ids=list(range(NCORES)))
    outs = [res.results[ci]["out"] for ci in range(NCORES)]
    return np.concatenate(outs, axis=0).astype(np.float32, copy=False)
```
